# Optimizing a Trainium2 kernel written in Bass

```python
import jax, jax.numpy as jnp
from jax import lax
import numpy as np

D_MODEL = 1024
BATCH = 32
SEQ = 2048
DEPTH = 4

PLE_DIM = 256
HEAD_DIM = 64
D_CONF = 3 * D_MODEL // 8
D_SHORT = 3 * D_MODEL // 8
D_POOL = D_MODEL - D_CONF - D_SHORT
D_IN = 2 * D_CONF + 3 * D_SHORT + D_POOL
IN_SPLITS = tuple(int(v) for v in np.cumsum([D_CONF, D_CONF, D_SHORT, D_SHORT, D_SHORT]))
CONF_KERNEL = 31
SHORT_KERNEL = 3
POOL_WINDOWS = (2, 4, 8, 16)
N_POOL_GROUPS = len(POOL_WINDOWS)
POOL_GROUP = D_POOL // N_POOL_GROUPS
N_EXPERTS = 32
TOP_K = 4
D_EXPERT = D_MODEL
SWIGLU_LIMIT = 7.0
SWIGLU_ALPHA = 1.702
MOE_BLOCK = 128
LN_EPS = 1e-5
DEEPNORM_ALPHA = float((2 * DEPTH) ** 0.25)
DEEPNORM_BETA = float((8 * DEPTH) ** -0.25)

kernel_name = "hybrid_conv_pool_moe_deepnorm"


def layer_norm(x, g, b):
    xf = x.astype(jnp.float32)
    mu = xf.mean(-1, keepdims=True)
    var = jnp.square(xf - mu).mean(-1, keepdims=True)
    y = (xf - mu) * lax.rsqrt(var + LN_EPS)
    return (y * g.astype(jnp.float32) + b.astype(jnp.float32)).astype(x.dtype)


def causal_depthwise_conv(u, w):
    k, c = w.shape
    return lax.conv_general_dilated(
        u, w[:, None, :].astype(u.dtype), window_strides=(1,), padding=[(k - 1, 0)],
        dimension_numbers=("NWC", "WIO", "NWC"), feature_group_count=c)


def multiscale_pool(v, pool_w, pool_scale):
    b, s, _ = v.shape
    vg = v.astype(jnp.float32).reshape(b, s, N_POOL_GROUPS, POOL_GROUP)
    cs = jnp.cumsum(vg, axis=1)
    means = []
    for g, w in enumerate(POOL_WINDOWS):
        c = cs[:, :, g]
        prev = jnp.pad(c[:, : s - w], ((0, 0), (w, 0), (0, 0)))
        cnt = jnp.minimum(jnp.arange(1, s + 1), w).astype(jnp.float32)[None, :, None]
        means.append((c - prev) / cnt)
    pooled = (jnp.stack(means, axis=2) - vg).astype(v.dtype)
    mixed = jnp.einsum("bsgc,gcd->bsgd", pooled, pool_w)
    return mixed.reshape(b, s, D_POOL) * pool_scale


def hybrid_mixer(x, w_in, conv_a_w, conv_a_b, ln_a_g, ln_a_b, conv_b_w, pool_w, pool_scale, w_out):
    z = x @ w_in
    a_val, a_gate, b_gate, c_gate, b_val, pool_in = jnp.split(z, IN_SPLITS, axis=-1)
    u = a_val * jax.nn.sigmoid(a_gate)
    u = causal_depthwise_conv(u, conv_a_w) + conv_a_b
    y_a = jax.nn.silu(layer_norm(u, ln_a_g, ln_a_b))
    y_b = b_gate * causal_depthwise_conv(c_gate * b_val, conv_b_w)
    y_c = multiscale_pool(pool_in, pool_w, pool_scale)
    return jnp.concatenate([y_a, y_b, y_c], axis=-1) @ w_out


def moe_ffn(h, router_w, router_b, w_gate_up, b_gate_up, w_down, b_down):
    bsz, s, d = h.shape
    n = bsz * s
    hf = h.reshape(n, d)
    logits = (hf @ router_w + router_b).astype(jnp.float32)
    top_logits, top_idx = lax.top_k(logits, TOP_K)
    gates = jax.nn.softmax(top_logits, axis=-1)
    e_flat = top_idx.reshape(-1)
    order = jnp.argsort(e_flat)
    e_sorted = e_flat[order]
    tok_sorted = order // TOP_K
    gate_sorted = gates.reshape(-1)[order].astype(h.dtype)
    counts = jnp.bincount(e_flat, length=N_EXPERTS)
    starts = jnp.cumsum(counts) - counts
    padded = (counts + MOE_BLOCK - 1) // MOE_BLOCK * MOE_BLOCK
    pad_ends = jnp.cumsum(padded)
    pad_starts = pad_ends - padded
    dest = pad_starts[e_sorted] + (jnp.arange(n * TOP_K) - starts[e_sorted])
    n_blocks = -(-(n * TOP_K) // MOE_BLOCK) + N_EXPERTS
    n_rows = n_blocks * MOE_BLOCK
    buf = jnp.zeros((n_rows, d), h.dtype).at[dest].set(hf[tok_sorted])
    block_start = jnp.arange(n_blocks) * MOE_BLOCK
    block_expert = jnp.minimum(jnp.searchsorted(pad_ends, block_start, side="right"), N_EXPERTS - 1)

    def expert_block(args):
        xb, e = args
        gu = xb @ w_gate_up[e] + b_gate_up[e]
        gate, up = jnp.split(gu, 2, axis=-1)
        gate = jnp.minimum(gate, SWIGLU_LIMIT)
        up = jnp.clip(up, -SWIGLU_LIMIT, SWIGLU_LIMIT)
        act = (up + 1.0) * gate * jax.nn.sigmoid(SWIGLU_ALPHA * gate)
        return act @ w_down[e] + b_down[e]

    yb = lax.map(expert_block, (buf.reshape(n_blocks, MOE_BLOCK, d), block_expert))
    y_sorted = yb.reshape(n_rows, d)[dest] * gate_sorted[:, None]
    out = jnp.zeros((n, d), h.dtype).at[tok_sorted].add(y_sorted)
    return out.reshape(bsz, s, d)


def setup_inputs(seed: int = 0) -> dict:
    key = jax.random.key(seed)
    ks = jax.random.split(key, 23)
    f32 = jnp.float32
    nrm = lambda k, shape, scale: jax.random.normal(k, shape, f32) * scale
    L = DEPTH
    return {
        "x": nrm(ks[0], (BATCH, SEQ, D_MODEL), 1.0),
        "p": nrm(ks[1], (DEPTH, BATCH, SEQ, PLE_DIM), 1.0),
        "w_in": nrm(ks[2], (L, D_MODEL, D_IN), D_MODEL ** -0.5),
        "conv_a_w": nrm(ks[3], (L, CONF_KERNEL, D_CONF), CONF_KERNEL ** -0.5),
        "conv_a_b": nrm(ks[4], (L, D_CONF), 0.02),
        "ln_a_g": 1.0 + nrm(ks[5], (L, D_CONF), 0.05),
        "ln_a_b": nrm(ks[6], (L, D_CONF), 0.02),
        "conv_b_w": nrm(ks[7], (L, SHORT_KERNEL, D_SHORT), SHORT_KERNEL ** -0.5),
        "pool_w": nrm(ks[8], (L, N_POOL_GROUPS, POOL_GROUP, POOL_GROUP), POOL_GROUP ** -0.5),
        "pool_scale": 1.0 + nrm(ks[9], (L, D_POOL), 0.1),
        "w_out": nrm(ks[10], (L, D_MODEL, D_MODEL), DEEPNORM_BETA * D_MODEL ** -0.5),
        "ln1_g": 1.0 + nrm(ks[11], (L, D_MODEL), 0.05),
        "ln1_b": nrm(ks[12], (L, D_MODEL), 0.02),
        "router_w": nrm(ks[13], (L, D_MODEL, N_EXPERTS), D_MODEL ** -0.5),
        "router_b": nrm(ks[14], (L, N_EXPERTS), 0.01),
        "w_gate_up": nrm(ks[15], (L, N_EXPERTS, D_MODEL, 2 * D_EXPERT), D_MODEL ** -0.5),
        "b_gate_up": nrm(ks[16], (L, N_EXPERTS, 2 * D_EXPERT), 0.02),
        "w_down": nrm(ks[17], (L, N_EXPERTS, D_EXPERT, D_MODEL), DEEPNORM_BETA * D_EXPERT ** -0.5),
        "b_down": nrm(ks[18], (L, N_EXPERTS, D_MODEL), 0.02),
        "ple_w_gate": nrm(ks[19], (L, D_MODEL, D_MODEL), D_MODEL ** -0.5),
        "ple_w_proj": nrm(ks[20], (L, PLE_DIM, D_MODEL), DEEPNORM_BETA * PLE_DIM ** -0.5),
        "ln2_g": 1.0 + nrm(ks[21], (L, D_MODEL), 0.05),
        "ln2_b": nrm(ks[22], (L, D_MODEL), 0.02),
    }


def reference(x, p, w_in, conv_a_w, conv_a_b, ln_a_g, ln_a_b, conv_b_w, pool_w, pool_scale, w_out,
              ln1_g, ln1_b, router_w, router_b, w_gate_up, b_gate_up, w_down, b_down,
              ple_w_gate, ple_w_proj, ln2_g, ln2_b):
    for i in range(DEPTH):
        mix = hybrid_mixer(x, w_in[i], conv_a_w[i], conv_a_b[i], ln_a_g[i], ln_a_b[i], conv_b_w[i],
                           pool_w[i], pool_scale[i], w_out[i])
        h = layer_norm(DEEPNORM_ALPHA * x + mix, ln1_g[i], ln1_b[i])
        ffn = moe_ffn(h, router_w[i], router_b[i], w_gate_up[i], b_gate_up[i], w_down[i], b_down[i])
        ple = jax.nn.sigmoid(h @ ple_w_gate[i]) * (p[i] @ ple_w_proj[i])
        x = layer_norm(DEEPNORM_ALPHA * h + ffn + ple, ln2_g[i], ln2_b[i])
    return x
```

```python
import contextlib
import numpy as np
import ml_dtypes
import concourse.bass as bass
import concourse.mybir as mybir
from concourse.bass_utils import run_bass_kernel_spmd

F32 = mybir.dt.float32
BF16 = mybir.dt.bfloat16
I32 = mybir.dt.int32
ALU = mybir.AluOpType
AF = mybir.ActivationFunctionType
AX = mybir.AxisListType

D = 1024
DIN = 2176
NCH = 17
E = 32
PLE = 256
TT = 256
SEQ = 2048
LN_EPS = 1e-5
NCORES = 8


class Sched:
    def __init__(self, nc, es):
        self.nc = nc
        self.es = es
        self.eng = {'pe': nc.tensor, 'dve': nc.vector, 'act': nc.scalar, 'pool': nc.gpsimd, 'sp': nc.sync}
        self.sem = {e: es.enter_context(nc.semaphore(f"sem_{e}")) for e in ('pe', 'dve', 'act', 'pool')}
        self.cnt = {e: 0 for e in ('pe', 'dve', 'act', 'pool')}
        self.dsem = {}
        self.dcnt = {}
        self.last_w = {}
        self.readers = {}
        self.waited = {}
        self.nwait = 0

    def _wait(self, eng, tok):
        kind, who, val = tok
        if kind == 'c' and who == 'pe' and eng == 'pe':
            return
        key = (eng, kind, who)
        if self.waited.get(key, 0) >= val:
            return
        semh = self.sem[who] if kind == 'c' else self.dsem[who]
        self.eng[eng].wait_ge(semh, val)
        self.waited[key] = val
        self.nwait += 1

    def _deps(self, eng, reads, writes):
        for k in reads:
            t = self.last_w.get(k)
            if t is not None:
                self._wait(eng, t)
        for k in writes:
            t = self.last_w.get(k)
            if t is not None:
                self._wait(eng, t)
            for (kind, who), val in self.readers.get(k, {}).items():
                self._wait(eng, (kind, who, val))

    def _commit(self, tok, reads, writes):
        kind, who, val = tok
        for k in reads:
            self.readers.setdefault(k, {})[(kind, who)] = val
        for k in writes:
            self.last_w[k] = tok
            self.readers[k] = {}

    def op(self, eng, fn, reads=(), writes=()):
        self._deps(eng, reads, writes)
        ins = fn(self.eng[eng])
        self.cnt[eng] += 1
        ins.then_inc(self.sem[eng], 1)
        self._commit(('c', eng, self.cnt[eng]), reads, writes)

    def pe(self, fns, reads=(), writes=()):
        self._deps('pe', reads, writes)
        ins = None
        for f in fns:
            ins = f(self.nc.tensor)
        self.cnt['pe'] += 1
        ins.then_inc(self.sem['pe'], 1)
        self._commit(('c', 'pe', self.cnt['pe']), reads, writes)

    def dma(self, q, group, fn, reads=(), writes=()):
        if group not in self.dsem:
            self.dsem[group] = self.es.enter_context(self.nc.semaphore(f"dsem_{group}"))
            self.dcnt[group] = 0
        self._deps(q, reads, writes)
        ins = fn(self.eng[q])
        self.dcnt[group] += 16
        ins.then_inc(self.dsem[group], 16)
        self._commit(('d', group, self.dcnt[group]), reads, writes)

    def group_sync(self, group, keys):
        tok = ('d', group, self.dcnt[group])
        for k in keys:
            self.last_w[k] = tok
            self.readers[k] = {}

    def barrier(self):
        toks = [('c', e, self.cnt[e]) for e in self.cnt if self.cnt[e] > 0]
        toks += [('d', g, self.dcnt[g]) for g in self.dcnt if self.dcnt[g] > 0]
        for e in ('pe', 'dve', 'act', 'pool', 'sp'):
            for t in toks:
                self._wait(e, t)
        self.last_w = {}
        self.readers = {}


def build_program(nseq, depth, capb, ub):
    NTOK = nseq * SEQ
    NT = NTOK // TT
    TPS = SEQ // TT
    NT128 = NTOK // 128
    CAPT = capb * 128
    NSLOT = E * CAPT + 128
    TRASH = E * CAPT
    NU = capb // ub
    US = ub * 128
    NSPL = (US + 511) // 512
    NW = US // NSPL
    assert NW * NSPL == US and capb % ub == 0
    ALPHA = float((2 * 4) ** 0.25)

    nc = bass.Bass("TRN2", target_bir_lowering=False)
    dt = lambda name, shape, dtype, kind: nc.dram_tensor(name, shape, dtype, kind=kind).ap()
    x_in = dt("x", [NTOK, D], F32, "ExternalInput")
    p_in = dt("p", [depth, NTOK, PLE], F32, "ExternalInput")
    w_in_d = dt("w_in", [depth, D, DIN], F32, "ExternalInput")
    w_out_d = dt("w_out", [depth, D, D], F32, "ExternalInput")
    pg_d = dt("ple_w_gate", [depth, D, D], F32, "ExternalInput")
    ppj_d = dt("ple_w_proj", [depth, PLE, D], F32, "ExternalInput")
    rw_d = dt("router_w", [depth, D, E], F32, "ExternalInput")
    rb_d = dt("router_b", [depth, E], F32, "ExternalInput")
    w1_d = dt("w_gate_up", [depth, E, D, 2 * D], F32, "ExternalInput")
    w2_d = dt("w_down", [depth, E, D, D], F32, "ExternalInput")
    b2_d = dt("b_down", [depth, E, D], F32, "ExternalInput")
    poolw_d = dt("pool_w", [depth, 4, 64, 64], F32, "ExternalInput")
    lnv_d = dt("lnv", [depth, 4, D], F32, "ExternalInput")
    NPP = 93 + 3 + 3 + 3 + 9 + 2 + 512
    pp_d = dt("pp", [depth, 128, NPP], F32, "ExternalInput")
    cst_f_d = dt("cst_f", [128, 128 + 32 + 32], F32, "ExternalInput")
    cst_b_d = dt("cst_b", [128, 3 * 128 + 2 * 16 * 128], BF16, "ExternalInput")
    out_d = dt("out", [NTOK, D], F32, "ExternalOutput")
    r_buf = dt("r_buf", [NTOK, D], F32, "Internal")
    hbuf = dt("hbuf", [NSLOT, D], BF16, "Internal")
    ybuf = dt("ybuf", [NSLOT, D], BF16, "Internal")

    es = contextlib.ExitStack()
    with es:
        S = Sched(nc, es)
        uid = [0]

        def sb(name, shape, dtype, stack=es):
            uid[0] += 1
            return stack.enter_context(nc.sbuf_tensor(f"s{uid[0]}_{name}", shape, dtype))

        def ps(name, shape, dtype, stack=es):
            uid[0] += 1
            return stack.enter_context(nc.psum_tensor(f"q{uid[0]}_{name}", shape, dtype))

        cst_f = sb("cst_f", [128, 192], F32)
        cst_b = sb("cst_b", [128, 3 * 128 + 2 * 16 * 128], BF16)
        ident_f = cst_f[:, 0:128]
        ebase1m = cst_f[:, 128:160]
        poolcorr = cst_f[:, 160:192]
        ident = cst_b[:, 0:128]
        ltri = cst_b[:, 128:256]
        ones = cst_b[:, 256:384]
        pooldg = cst_b[:, 384:384 + 4096]
        dest_all = sb("dest_all", [128, NT128, 4], I32)
        gate_all = sb("gate_all", [128, NT128, 4], F32)
        run_cnt = sb("run_cnt", [128, E], F32)
        epsc = sb("epsc", [128, 1], F32)
        S.op('dve', lambda v: v.memset(epsc[:], LN_EPS), writes=['epsc'])
        ppar = sb("ppar", [128, NPP], F32)
        b1p1 = sb("b1p1", [128, E, 8], F32)

        S.dma('sp', 'cst', lambda q: q.dma_start(out=cst_f[:], in_=cst_f_d), writes=['cst'])
        S.dma('sp', 'cst', lambda q: q.dma_start(out=cst_b[:], in_=cst_b_d), writes=['cst'])
        with contextlib.ExitStack() as p0:
            zrow = sb("zrow", [128, D], BF16, p0)
            S.op('dve', lambda v: v.memset(zrow[:], 0.0), writes=['zrow'])
            S.dma('sp', 'cst', lambda q: q.dma_start(out=ybuf[TRASH:TRASH + 128, :], in_=zrow[:]),
                  reads=['zrow'], writes=['ybuf_trash'])
            S.barrier()

        caw = ppar[:, 0:93]
        cab = ppar[:, 93:96]
        lag = ppar[:, 96:99]
        lab = ppar[:, 99:102]
        cbw = ppar[:, 102:111]
        psc = ppar[:, 111:113]
        b1 = ppar[:, 113:113 + 512]

        for l in range(depth):
            src_x = x_in if l == 0 else r_buf
            dst_x = out_d if l == depth - 1 else r_buf
            S.dma('sp', 'par_pp', lambda q: q.dma_start(out=ppar[:], in_=pp_d[l]), writes=['ppar'])
            S.op('dve', lambda v: v.tensor_scalar(
                out=b1p1[:], in0=b1.rearrange("p (e c) -> p e c", c=16)[:, :, 8:16], scalar1=1.0, scalar2=None,
                op0=ALU.add), reads=['ppar'], writes=['b1p1'])
            S.op('dve', lambda v: v.memset(run_cnt[:], 0.0), writes=['run_cnt'])

            with contextlib.ExitStack() as pa:
                lnbc = sb("lnbc1", [128, 2, D], F32, pa)
                S.dma('sp', 'par_ln', lambda q: q.dma_start(
                    out=lnbc[:].rearrange("p a d -> p (a d)"),
                    in_=lnv_d[l, 0:2].rearrange("a d -> (a d)").partition_broadcast(128)), writes=['lnbc'])
                w_in = sb("w_in", [128, 8, DIN], BF16, pa)
                w_out = sb("w_out", [128, 8, D], BF16, pa)
                pgw = sb("pgw", [128, 8, D], BF16, pa)
                ppw = sb("ppw", [128, 2, D], BF16, pa)
                rw = sb("rw", [128, 8, E], BF16, pa)
                rbb = sb("rbb", [128, E], F32, pa)
                dgA = sb("dgA", [128, 93, 128], BF16, pa)
                pwbd = sb("pwbd", [128, 2, 128], BF16, pa)
                x_tm = [sb("x_tm0", [128, 2, D], F32, pa)]
                xres = [sb(f"xres{i}", [128, D], F32, pa) for i in range(2)]
                p_tm = [sb("p_tm0", [128, 2, PLE], F32, pa)] * 2
                xb = sb("xb", [128, 2, D], BF16, pa)
                xT = sb("xT", [128, 8, TT], BF16, pa)
                sg = sb("sg", [128, TT], F32, pa)
                uh = sb("uh", [128, 3, 30 + TT], BF16, pa)
                wh = sb("wh", [128, 3, 2 + TT], F32, pa)
                bgt = sb("bgt", [128, 1, TT], F32, pa)
                cgt = sg
                tb = sb("tb", [128, TT], F32, pa)
                ph = sb("ph", [128, 2, 15 + TT], BF16, pa)
                pooled = sb("pooled", [128, 2, TT], BF16, pa)
                v32 = sb("v32", [128, 3, TT], F32, pa)
                vb = sb("vb", [128, 3, TT], BF16, pa)
                vsq = sb("vsq", [128, 3, TT], BF16, pa)
                mean_sb = sb("mean_sb", [128, TT], F32, pa)
                var_sb = sb("var_sb", [128, TT], F32, pa)
                rstd = sb("rstd", [128, TT], F32, pa)
                tn = tb
                yT = [sb(f"yT{i}", [128, 8, TT], BF16, pa) for i in range(2)]
                sbuf_s = sb("s_s", [128, D], F32, pa)
                h32 = sb("h32", [128, D], F32, pa)
                hb = [sb(f"hb{i}", [128, D], BF16, pa) for i in range(2)]
                hT = sb("hT", [128, 8, TT], BF16, pa)
                pb = sb("pb", [128, 2, PLE], BF16, pa)
                pT = sb("pT", [128, 2, TT], BF16, pa)
                sgm = sbuf_s
                rt = [sb("rt0", [128, D], F32, pa)] * 2
                st6 = sb("st6", [128, 2, 6], F32, pa)
                mv = sb("mv", [128, 2], F32, pa)
                rs1 = sb("rs1", [128, 1], F32, pa)
                lg = sb("lg", [128, E], F32, pa)
                mx8 = sb("mx8", [128, 8], F32, pa)
                msk = sb("msk", [128, E], F32, pa)
                mskb = sb("mskb", [128, E], BF16, pa)
                nmx = sb("nmx", [128, 1], F32, pa)
                ex = sb("ex", [128, E], F32, pa)
                ssum = sb("ssum", [128, 1], F32, pa)
                G = sb("G", [128, E], F32, pa)
                Cr = sb("Cr", [128, E], F32, pa)
                okm = sb("okm", [128, E], F32, pa)
                Vv = sb("Vv", [128, E], F32, pa)
                d8 = sb("d8", [128, 8], F32, pa)
                eqg = sb("eqg", [128, E], F32, pa)
                psT = [ps(f"psT{i}", [128, D], BF16, pa) for i in range(2)]
                psA = [ps(f"psA{i}", [128, 512], F32, pa) for i in range(4)]
                psB = [ps(f"psB{i}", [128, 512], F32, pa) for i in range(2)]

                for hlf in range(2):
                    S.dma('pool', 'wA', lambda q: q.dma_start(
                        out=w_in[:, :, hlf * 1088:(hlf + 1) * 1088],
                        in_=w_in_d[l].rearrange("(k p) n -> p k n", p=128)[:, :, hlf * 1088:(hlf + 1) * 1088]),
                        writes=['w_in'])
                S.dma('pool', 'wA', lambda q: q.dma_start(
                    out=w_out[:], in_=w_out_d[l].rearrange("(k p) n -> p k n", p=128)), writes=['w_out'])
                S.dma('pool', 'wA', lambda q: q.dma_start(
                    out=pgw[:], in_=pg_d[l].rearrange("(k p) n -> p k n", p=128)), writes=['pgw'])
                S.dma('pool', 'wA', lambda q: q.dma_start(
                    out=ppw[:], in_=ppj_d[l].rearrange("(k p) n -> p k n", p=128)), writes=['ppw'])
                S.dma('pool', 'wA', lambda q: q.dma_start(
                    out=rw[:], in_=rw_d[l].rearrange("(k p) n -> p k n", p=128)), writes=['rw'])
                S.dma('sp', 'par_rb', lambda q: q.dma_start(out=rbb[:], in_=rb_d[l].partition_broadcast(128)),
                      writes=['rbb'])
                S.op('dve', lambda v: v.memset(pwbd[:], 0.0), writes=['pwbd'])
                for g in range(4):
                    j, o = g // 2, (g % 2) * 64
                    S.dma('pool', 'wA', lambda q: q.dma_start(out=pwbd[o:o + 64, j, o:o + 64], in_=poolw_d[l, g]),
                          reads=[], writes=['pwbd'])
                S.group_sync('wA', ['w_in', 'w_out', 'pgw', 'ppw', 'rw', 'pwbd'])
                for jk in range(93):
                    S.op('dve', lambda v: v.tensor_scalar(out=dgA[:, jk, :], in0=ident_f, scalar1=caw[:, jk:jk + 1],
                                                          scalar2=None, op0=ALU.mult),
                         reads=['ppar', 'cst'], writes=['dgA'])

                def load_x(it_):
                    S.dma('sp', "x_tm0", lambda q: q.dma_start(
                        out=x_tm[0][:],
                        in_=src_x[it_ * TT:(it_ + 1) * TT, :].rearrange("(s p) d -> p s d", p=128)),
                        reads=[], writes=["x_tm0"])

                def zmm(c, bank):
                    S.pe([lambda t, k=k: t.matmul(psA[bank][:, 0:TT], w_in[:, k, c * 128:(c + 1) * 128],
                                                  xT[:, k, :], start=(k == 0), stop=(k == 7))
                          for k in range(8)], reads=['w_in', 'xT'], writes=[f"psA{bank}"])

                def front_steps(it):
                    ts_ = it % TPS
                    ysl = it % 2
                    yTc = yT[ysl]
                    kyT = f"yT{ysl}"
                    steps = {}

                    def f_xc():
                        S.op('act', lambda a: a.copy(out=xb[:], in_=x_tm[0][:]), reads=["x_tm0"], writes=['xb'])
                        if it + 1 < NT:
                            load_x(it + 1)
                    steps['XC'] = f_xc

                    def f_xT():
                        for s in range(2):
                            S.pe([lambda t, k=k: t.transpose(out=psT[s][:, k * 128:(k + 1) * 128],
                                                             in_=xb[:, s, k * 128:(k + 1) * 128], identity=ident)
                                  for k in range(8)], reads=['xb'], writes=[f"psT{s}"])
                            S.op('dve', lambda v: v.tensor_copy(out=xT[:, :, s * 128:(s + 1) * 128],
                                                                in_=psT[s][:].rearrange("p (k t) -> p k t", t=128)),
                                 reads=[f"psT{s}"], writes=['xT'])
                        if ts_ == 0:
                            S.op('pool', lambda g_: g_.memset(uh[:, :, 0:30], 0.0), writes=['uh0', 'uh1', 'uh2'])
                            S.op('pool', lambda g_: g_.memset(wh[:, :, 0:2], 0.0), writes=['wh0', 'wh1', 'wh2'])
                            S.op('pool', lambda g_: g_.memset(ph[:, :, 0:15], 0.0), writes=['ph0', 'ph1'])
                        else:
                            S.op('pool', lambda g_: g_.tensor_copy(out=uh[:, :, 0:30], in_=uh[:, :, TT:TT + 30]),
                                 reads=['uh0', 'uh1', 'uh2'], writes=['uh0', 'uh1', 'uh2'])
                            S.op('pool', lambda g_: g_.tensor_copy(out=wh[:, :, 0:2], in_=wh[:, :, TT:TT + 2]),
                                 reads=['wh0', 'wh1', 'wh2'], writes=['wh0', 'wh1', 'wh2'])
                            S.op('pool', lambda g_: g_.tensor_copy(out=ph[:, :, 0:15], in_=ph[:, :, TT:TT + 15]),
                                 reads=['ph0', 'ph1'], writes=['ph0', 'ph1'])
                    steps['X'] = f_xT

                    def f_A(j):
                        def f():
                            zmm(3 + j, 0)
                            S.op('act', lambda a: a.activation(out=sg[:], in_=psA[0][:, 0:TT], func=AF.Sigmoid),
                                 reads=['psA0'], writes=['sg'])
                            zmm(j, 1)
                            S.op('dve', lambda v: v.tensor_tensor(out=uh[:, j, 30:30 + TT], in0=psA[1][:, 0:TT],
                                                                  in1=sg[:], op=ALU.mult),
                                 reads=['psA1', 'sg'], writes=[f"uh{j}"])
                        return f
                    for j in range(3):
                        steps[f'A{j}'] = f_A(j)

                    def f_conv(j):
                        def f():
                            bank = j % 2
                            S.pe([lambda t, k=k: t.matmul(psA[bank][:, 0:TT], dgA[:, j * 31 + k, :],
                                                          uh[:, j, k:k + TT], start=(k == 0), stop=(k == 30))
                                  for k in range(31)], reads=['dgA', f"uh{j}"], writes=[f"psA{bank}"])
                            S.op('act', lambda a: a.activation(out=v32[:, j, :], in_=psA[bank][:, 0:TT],
                                                               func=AF.Identity, bias=cab[:, j:j + 1]),
                                 reads=[f"psA{bank}", 'ppar'], writes=[f"v32_{j}"])
                            S.op('act', lambda a: a.activation(out=vsq[:, j, :], in_=psA[bank][:, 0:TT],
                                                               func=AF.Square, bias=cab[:, j:j + 1]),
                                 reads=[f"psA{bank}", 'ppar'], writes=[f"vsq{j}"])
                            S.op('pool', lambda g_: g_.tensor_copy(out=vb[:, j, :], in_=v32[:, j, :]),
                                 reads=[f"v32_{j}"], writes=[f"vb{j}"])
                        return f

                    def f_B(j):
                        def f():
                            zmm(6 + j, 2)
                            S.op('act', lambda a: a.copy(out=bgt[:, 0, :], in_=psA[2][:, 0:TT]),
                                 reads=['psA2'], writes=['bgt'])
                            zmm(9 + j, 3)
                            S.op('act', lambda a: a.copy(out=cgt[:], in_=psA[3][:, 0:TT]),
                                 reads=['psA3'], writes=['sg'])
                            zmm(12 + j, 2)
                            S.op('dve', lambda v: v.tensor_tensor(out=wh[:, j, 2:2 + TT], in0=psA[2][:, 0:TT],
                                                                  in1=cgt[:], op=ALU.mult),
                                 reads=['psA2', 'sg'], writes=[f"wh{j}"])
                            S.op('dve', lambda v: v.tensor_scalar(out=tb[:], in0=wh[:, j, 2:2 + TT],
                                                                  scalar1=cbw[:, j * 3 + 2:j * 3 + 3], scalar2=None,
                                                                  op0=ALU.mult), reads=[f"wh{j}", 'ppar'], writes=['tb'])
                            for k in (1, 0):
                                S.op('dve', lambda v: v.scalar_tensor_tensor(
                                    out=tb[:], in0=wh[:, j, k:k + TT], scalar=cbw[:, j * 3 + k:j * 3 + k + 1],
                                    in1=tb[:], op0=ALU.mult, op1=ALU.add), reads=[f"wh{j}", 'tb', 'ppar'], writes=['tb'])
                            S.op('dve', lambda v: v.tensor_tensor(out=yTc[:, 3 + j, :], in0=tb[:], in1=bgt[:, 0, :],
                                                                  op=ALU.mult), reads=['tb', 'bgt'], writes=[f"{kyT}_{3 + j}"])
                        return f
                    for j in range(3):
                        steps[f'C{j}'] = f_conv(j)
                        steps[f'B{j}'] = f_B(j)

                    def f_pool():
                        for j in range(2):
                            zmm(15 + j, 3)
                            S.op('act', lambda a: a.copy(out=ph[:, j, 15:15 + TT], in_=psA[3][:, 0:TT]),
                                 reads=['psA3'], writes=[f"ph{j}"])
                        for j in range(2):
                            ntap = 4 if j == 0 else 16
                            S.pe([lambda t, k=k: t.matmul(
                                psA[j][:, 0:TT], pooldg[:, (j * 16 + k) * 128:(j * 16 + k + 1) * 128],
                                ph[:, j, 15 - k:15 - k + TT], start=(k == 0), stop=(k == ntap - 1))
                                for k in range(ntap)], reads=[f"ph{j}", 'cst'], writes=[f"psA{j}"])
                            S.op('dve', lambda v: v.tensor_tensor(out=pooled[:, j, :], in0=psA[j][:, 0:TT],
                                                                  in1=ph[:, j, 15:15 + TT], op=ALU.subtract),
                                 reads=[f"psA{j}", f"ph{j}"], writes=[f"pooled{j}"])
                            if ts_ == 0:
                                S.op('dve', lambda v: v.tensor_tensor(out=tb[:, 0:16], in0=psA[j][:, 0:16],
                                                                      in1=poolcorr[:, j * 16:(j + 1) * 16],
                                                                      op=ALU.mult),
                                     reads=[f"psA{j}", 'cst'], writes=['tb'])
                                S.op('dve', lambda v: v.tensor_tensor(out=pooled[:, j, 0:16], in0=tb[:, 0:16],
                                                                      in1=ph[:, j, 15:31], op=ALU.subtract),
                                     reads=['tb', f"ph{j}"], writes=[f"pooled{j}"])
                        for j in range(2):
                            S.pe([lambda t: t.matmul(psA[2 + j][:, 0:TT], pwbd[:, j, :], pooled[:, j, :],
                                                     start=True, stop=True)],
                                 reads=['pwbd', f"pooled{j}"], writes=[f"psA{2 + j}"])
                            S.op('act', lambda a: a.activation(out=yTc[:, 6 + j, :], in_=psA[2 + j][:, 0:TT],
                                                               func=AF.Identity, scale=psc[:, j:j + 1]),
                                 reads=[f"psA{2 + j}", 'ppar'], writes=[f"{kyT}_{6 + j}"])
                    steps['PL'] = f_pool

                    def f_lna():
                        S.pe([lambda t, j=j: t.matmul(psA[2][:, 0:TT], ones, vb[:, j, :], start=(j == 0), stop=(j == 2))
                              for j in range(3)], reads=['vb0', 'vb1', 'vb2', 'cst'], writes=['psA2'])
                        S.pe([lambda t, j=j: t.matmul(psA[3][:, 0:TT], ones, vsq[:, j, :], start=(j == 0), stop=(j == 2))
                              for j in range(3)], reads=['vsq0', 'vsq1', 'vsq2', 'cst'], writes=['psA3'])
                        S.op('act', lambda a: a.activation(out=mean_sb[:], in_=psA[2][:, 0:TT], func=AF.Identity,
                                                           scale=1.0 / 384), reads=['psA2'], writes=['mean_sb'])
                        S.op('dve', lambda v: v.tensor_tensor(out=var_sb[:], in0=mean_sb[:], in1=mean_sb[:], op=ALU.mult),
                             reads=['mean_sb'], writes=['var_sb'])
                        S.op('dve', lambda v: v.scalar_tensor_tensor(out=var_sb[:], in0=psA[3][:, 0:TT], scalar=1.0 / 384,
                                                                     in1=var_sb[:], op0=ALU.mult, op1=ALU.subtract),
                             reads=['psA3', 'var_sb'], writes=['var_sb'])
                        S.op('act', lambda a: a.activation(out=rstd[:], in_=var_sb[:], func=AF.Sqrt, bias=epsc[:, 0:1]),
                             reads=['var_sb'], writes=['rstd'])
                        S.op('dve', lambda v: v.reciprocal(out=rstd[:], in_=rstd[:]), reads=['rstd'], writes=['rstd'])
                        for j in range(3):
                            S.op('pool', lambda g_: g_.tensor_tensor(out=tn[:], in0=v32[:, j, :], in1=mean_sb[:],
                                                                     op=ALU.subtract),
                                 reads=[f"v32_{j}", 'mean_sb'], writes=['tb'])
                            S.op('dve', lambda v: v.tensor_tensor(out=tn[:], in0=tn[:], in1=rstd[:], op=ALU.mult),
                                 reads=['tb', 'rstd'], writes=['tb'])
                            S.op('act', lambda a: a.activation(out=yTc[:, j, :], in_=tn[:], func=AF.Silu,
                                                               scale=lag[:, j:j + 1], bias=lab[:, j:j + 1]),
                                 reads=['tb', 'ppar'], writes=[f"{kyT}_{j}"])
                    steps['LN'] = f_lna
                    return steps

                def back_steps(it):
                    ysl = it % 2
                    yTc = yT[ysl]
                    kyT = f"yT{ysl}"
                    tok0 = it * TT

                    def b_prefetch():
                        S.dma('sp', "p_tm0", lambda q: q.dma_start(
                            out=p_tm[0][:], in_=p_in[l, tok0:tok0 + TT, :].rearrange("(s p) d -> p s d", p=128)),
                            writes=["p_tm0"])
                        for s in range(2):
                            S.dma('sp', f"xres{s}", lambda q: q.dma_start(
                                out=xres[s][:], in_=src_x[tok0 + s * 128:tok0 + (s + 1) * 128, :]),
                                writes=[f"xres{s}"])

                    def b_load():
                        S.op('act', lambda a: a.copy(out=pb[:], in_=p_tm[0][:]), reads=["p_tm0"], writes=['pb'])

                    def b_mix(s):
                        def f():
                            for hf in range(2):
                                S.pe([lambda t, k=k: t.matmul(psB[hf][:], yTc[:, k, s * 128:(s + 1) * 128],
                                                              w_out[:, k, hf * 512:(hf + 1) * 512],
                                                              start=(k == 0), stop=(k == 7))
                                      for k in range(8)], reads=[f"{kyT}_{c}" for c in range(8)] + ['w_out'], writes=[f"psB{hf}"])
                                S.op('dve', lambda v: v.scalar_tensor_tensor(
                                    out=sbuf_s[:, hf * 512:(hf + 1) * 512], in0=xres[s][:, hf * 512:(hf + 1) * 512],
                                    scalar=ALPHA, in1=psB[hf][:], op0=ALU.mult, op1=ALU.add),
                                    reads=[f"xres{s}", f"psB{hf}"], writes=['s_s'])
                                S.op('dve', lambda v: v.bn_stats(out=st6[:, hf, :],
                                                                 in_=sbuf_s[:, hf * 512:(hf + 1) * 512]),
                                     reads=['s_s'], writes=['st6'])
                            S.op('dve', lambda v: v.bn_aggr(out=mv[:], in_=st6[:].rearrange("p a b -> p (a b)")),
                                 reads=['st6'], writes=['mv'])
                        return f

                    def b_mixb(s):
                        def f():
                            S.op('act', lambda a: a.activation(out=rs1[:], in_=mv[:, 1:2], func=AF.Sqrt,
                                                               bias=epsc[:, 0:1]), reads=['mv'], writes=['rs1'])
                            S.op('dve', lambda v: v.reciprocal(out=rs1[:], in_=rs1[:]), reads=['rs1'], writes=['rs1'])
                            S.op('dve', lambda v: v.tensor_scalar(out=sbuf_s[:], in0=sbuf_s[:], scalar1=mv[:, 0:1],
                                                                  scalar2=rs1[:, 0:1], op0=ALU.subtract, op1=ALU.mult),
                                 reads=['s_s', 'mv', 'rs1'], writes=['s_s'])
                            S.op('pool', lambda g_: g_.tensor_tensor(out=sbuf_s[:], in0=sbuf_s[:], in1=lnbc[:, 0, :],
                                                                     op=ALU.mult), reads=['s_s', 'lnbc'], writes=['s_s'])
                            S.op('pool', lambda g_: g_.tensor_tensor(out=h32[:], in0=sbuf_s[:], in1=lnbc[:, 1, :],
                                                                     op=ALU.add), reads=['s_s', 'lnbc'], writes=['h32'])
                        return f

                    def b_route(s):
                        def f():
                            t128 = it * 2 + s
                            hs = t128 % 2
                            khb = f"hb{hs}"
                            S.op('act', lambda a: a.copy(out=hb[hs][:], in_=h32[:]), reads=['h32'], writes=[khb])
                            S.pe([lambda t, k=k: t.transpose(out=psT[s][:, k * 128:(k + 1) * 128],
                                                             in_=hb[hs][:, k * 128:(k + 1) * 128], identity=ident)
                                  for k in range(8)], reads=[khb], writes=[f"psT{s}"])
                            S.op('act', lambda a: a.copy(out=hT[:, :, s * 128:(s + 1) * 128],
                                                         in_=psT[s][:].rearrange("p (k t) -> p k t", t=128)),
                                 reads=[f"psT{s}"], writes=['hT'])
                        return f

                    def b_route_b(s):
                        def f():
                            t128 = it * 2 + s
                            S.pe([lambda t, k=k: t.matmul(psA[0][:, 0:E], hT[:, k, s * 128:(s + 1) * 128], rw[:, k, :],
                                                          start=(k == 0), stop=(k == 7)) for k in range(8)],
                                 reads=['hT', 'rw'], writes=['psA0'])
                            S.op('dve', lambda v: v.tensor_tensor(out=lg[:], in0=psA[0][:, 0:E], in1=rbb[:], op=ALU.add),
                                 reads=['psA0', 'rbb'], writes=['lg'])
                            S.op('dve', lambda v: v.max(out=mx8[:], in_=lg[:]), reads=['lg'], writes=['mx8'])
                            S.op('dve', lambda v: v.tensor_scalar(out=msk[:], in0=lg[:], scalar1=mx8[:, 3:4],
                                                                  scalar2=None, op0=ALU.is_ge),
                                 reads=['lg', 'mx8'], writes=['msk'])
                            S.op('dve', lambda v: v.tensor_scalar(out=nmx[:], in0=mx8[:, 0:1], scalar1=-1.0,
                                                                  scalar2=None, op0=ALU.mult),
                                 reads=['mx8'], writes=['nmx'])
                            S.op('act', lambda a: a.activation(out=ex[:], in_=lg[:], func=AF.Exp, bias=nmx[:, 0:1]),
                                 reads=['lg', 'nmx'], writes=['ex'])
                            S.op('pool', lambda g_: g_.tensor_copy(out=mskb[:], in_=msk[:]),
                                 reads=['msk'], writes=['mskb'])
                        return f

                    def b_route_c(s):
                        def f():
                            t128 = it * 2 + s
                            hs = t128 % 2
                            khb = f"hb{hs}"
                            S.pe([lambda t: t.matmul(psA[1][:, 0:E], ltri, mskb[:], start=True, stop=True)],
                                 reads=['mskb', 'cst'], writes=['psA1'])
                            S.pe([lambda t: t.matmul(psA[2][:, 0:E], ones, mskb[:], start=True, stop=True)],
                                 reads=['mskb', 'cst'], writes=['psA2'])
                            S.op('dve', lambda v: v.tensor_tensor(out=ex[:], in0=ex[:], in1=msk[:], op=ALU.mult),
                                 reads=['ex', 'msk'], writes=['ex'])
                            S.op('dve', lambda v: v.tensor_reduce(out=ssum[:], in_=ex[:], axis=AX.X, op=ALU.add),
                                 reads=['ex'], writes=['ssum'])
                            S.op('dve', lambda v: v.reciprocal(out=ssum[:], in_=ssum[:]),
                                 reads=['ssum'], writes=['ssum'])
                            S.op('dve', lambda v: v.tensor_scalar(out=G[:], in0=ex[:], scalar1=ssum[:, 0:1],
                                                                  scalar2=None, op0=ALU.mult),
                                 reads=['ex', 'ssum'], writes=['G'])
                            S.op('dve', lambda v: v.tensor_tensor(out=Cr[:], in0=psA[1][:, 0:E], in1=run_cnt[:],
                                                                  op=ALU.add), reads=['psA1', 'run_cnt'], writes=['Cr'])
                            S.op('dve', lambda v: v.tensor_tensor(out=run_cnt[:], in0=psA[2][:, 0:E], in1=run_cnt[:],
                                                                  op=ALU.add),
                                 reads=['psA2', 'run_cnt'], writes=['run_cnt'])
                            S.op('dve', lambda v: v.tensor_scalar(out=okm[:], in0=Cr[:], scalar1=float(CAPT),
                                                                  scalar2=None, op0=ALU.is_lt),
                                 reads=['Cr'], writes=['okm'])
                            S.op('dve', lambda v: v.tensor_tensor(out=Cr[:], in0=Cr[:], in1=ebase1m, op=ALU.add),
                                 reads=['Cr', 'cst'], writes=['Cr'])
                            S.op('dve', lambda v: v.tensor_tensor(out=Cr[:], in0=Cr[:], in1=okm[:], op=ALU.mult),
                                 reads=['Cr', 'okm'], writes=['Cr'])
                            S.op('dve', lambda v: v.scalar_tensor_tensor(out=Vv[:], in0=Cr[:], scalar=float(TRASH + 1),
                                                                         in1=msk[:], op0=ALU.add, op1=ALU.mult),
                                 reads=['Cr', 'msk'], writes=['Vv'])
                            S.op('dve', lambda v: v.max(out=d8[:], in_=Vv[:]), reads=['Vv'], writes=['d8'])
                            S.op('dve', lambda v: v.tensor_scalar(out=dest_all[:, t128, :], in0=d8[:, 0:4], scalar1=-1.0,
                                                                  scalar2=None, op0=ALU.add),
                                 reads=['d8'], writes=[f"dest{t128}"])
                            for k in range(4):
                                S.dma('pool', khb, lambda q: q.indirect_dma_start(
                                    out=hbuf,
                                    out_offset=bass.IndirectOffsetOnAxis(ap=dest_all[:, t128, k:k + 1], axis=0),
                                    in_=hb[hs][:], in_offset=None), reads=[khb, f"dest{t128}"], writes=[])
                            for k in range(4):
                                S.op('dve', lambda v: v.scalar_tensor_tensor(out=eqg[:], in0=Vv[:],
                                                                             scalar=d8[:, k:k + 1], in1=G[:],
                                                                             op0=ALU.is_equal, op1=ALU.mult),
                                     reads=['Vv', 'd8', 'G'], writes=['eqg'])
                                S.op('dve', lambda v: v.tensor_reduce(out=gate_all[:, t128, k:k + 1], in_=eqg[:],
                                                                      axis=AX.X, op=ALU.add),
                                     reads=['eqg'], writes=[f"gate{t128}"])
                        return f

                    def b_ple(s):
                        def f():
                            t128 = it * 2 + s
                            S.pe([lambda t, j=j: t.transpose(out=psT[s][:, j * 128:(j + 1) * 128],
                                                             in_=pb[:, s, j * 128:(j + 1) * 128], identity=ident)
                                  for j in range(2)], reads=['pb'], writes=[f"psT{s}"])
                            S.op('act', lambda a: a.copy(out=pT[:, :, s * 128:(s + 1) * 128],
                                                         in_=psT[s][:, 0:256].rearrange("p (k t) -> p k t", t=128)),
                                 reads=[f"psT{s}"], writes=['pT'])
                            for hf in range(2):
                                S.pe([lambda t, k=k: t.matmul(psB[hf][:], hT[:, k, s * 128:(s + 1) * 128],
                                                              pgw[:, k, hf * 512:(hf + 1) * 512],
                                                              start=(k == 0), stop=(k == 7)) for k in range(8)],
                                     reads=['hT', 'pgw'], writes=[f"psB{hf}"])
                                S.pe([lambda t, j=j: t.matmul(psA[2 + hf][:], pT[:, j, s * 128:(s + 1) * 128],
                                                              ppw[:, j, hf * 512:(hf + 1) * 512],
                                                              start=(j == 0), stop=(j == 1)) for j in range(2)],
                                     reads=['pT', 'ppw'], writes=[f"psA{2 + hf}"])
                                S.op('act', lambda a: a.activation(out=sgm[:, hf * 512:(hf + 1) * 512], in_=psB[hf][:],
                                                                   func=AF.Sigmoid), reads=[f"psB{hf}"], writes=['s_s'])
                                S.op('dve', lambda v: v.tensor_tensor(out=sgm[:, hf * 512:(hf + 1) * 512],
                                                                      in0=sgm[:, hf * 512:(hf + 1) * 512],
                                                                      in1=psA[2 + hf][:], op=ALU.mult),
                                     reads=['s_s', f"psA{2 + hf}"], writes=['s_s'])
                            S.op('dve', lambda v: v.scalar_tensor_tensor(out=rt[0][:], in0=h32[:], scalar=ALPHA,
                                                                         in1=sgm[:], op0=ALU.mult, op1=ALU.add),
                                 reads=['h32', 's_s'], writes=["rt0"])
                            S.dma('sp', "rt0", lambda q: q.dma_start(out=r_buf[t128 * 128:(t128 + 1) * 128, :],
                                                                     in_=rt[0][:]),
                                  reads=["rt0", f"xres{s}"], writes=[])
                        return f
                    return {'BL': b_load, 'PF': b_prefetch, 'M0': b_mix(0), 'M0b': b_mixb(0), 'M1b': b_mixb(1), 'R0a': b_route(0),
                            'R0b': b_route_b(0), 'R0c': b_route_c(0), 'P0': b_ple(0), 'M1': b_mix(1),
                            'R1a': b_route(1), 'R1b': b_route_b(1), 'R1c': b_route_c(1), 'P1': b_ple(1)}

                ORDER = ['BL', 'X', 'A0', 'A1', 'A2', 'M0', 'B0', 'M0b', 'B1', 'C0', 'R0a', 'R0b', 'P0', 'R0c', 'M1',
                         'B2', 'M1b', 'XC2', 'C1', 'C2', 'R1a', 'PL', 'R1b', 'P1', 'LN', 'R1c']
                load_x(0)
                fr = front_steps(0)
                fr['XC']()
                back_steps(0)['PF']()
                for nm in ORDER:
                    if nm in fr:
                        fr[nm]()
                if NT > 1:
                    front_steps(1)['XC']()
                for it in range(NT):
                    fr = front_steps(it + 1) if it + 1 < NT else {}
                    bk = back_steps(it)
                    for nm in ORDER:
                        if nm == 'XC2':
                            if it + 2 < NT:
                                front_steps(it + 2)['XC']()
                        elif nm in bk:
                            bk[nm]()
                        elif nm in fr:
                            fr[nm]()
                    if it + 1 < NT:
                        back_steps(it + 1)['PF']()
                S.barrier()

            with contextlib.ExitStack() as pbk:
                w1 = [sb(f"w1_{i}", [128, 8, 2 * D], BF16, pbk) for i in range(2)]
                w2 = [sb(f"w2_{i}", [128, 8, D], BF16, pbk) for i in range(2)]
                b2b = [sb(f"b2b{i}", [128, D], F32, pbk) for i in range(2)]
                rows = [sb(f"rows{i}", [128, D], BF16, pbk) for i in range(ub)]
                hTg = [sb(f"hTg{i}", [128, 8, US], BF16, pbk) for i in range(2)]
                actT = sb("actT", [128, 8, US], BF16, pbk)
                g2 = [sb(f"g2_{i}", [128, NSPL, NW], F32, pbk) for i in range(2)]
                sgx = [sb(f"sgx{i}", [128, NSPL, NW], F32, pbk) for i in range(2)]
                tg = [sb(f"tg{i}", [128, NSPL, NW], F32, pbk) for i in range(2)]
                au = [sb(f"au{i}", [128, NSPL, NW], F32, pbk) for i in range(2)]
                yst = [sb(f"yst{i}", [128, D], BF16, pbk) for i in range(2)]
                psT = [ps(f"psTb{i}", [128, D], BF16, pbk) for i in range(2)]
                psG = ps("psG", [128, NSPL, 512], F32, pbk)
                psU = ps("psU", [128, NSPL, 512], F32, pbk)
                psY = [ps(f"psY{i}", [128, 512], F32, pbk) for i in range(2)]
                def load_w(e_):
                    ws_ = e_ % 2
                    S.dma('pool', f"w1_{ws_}", lambda q: q.dma_start(
                        out=w1[ws_][:], in_=w1_d[l, e_].rearrange("(k p) n -> p k n", p=128)), writes=[f"w1_{ws_}"])
                    S.dma('pool', f"w2_{ws_}", lambda q: q.dma_start(
                        out=w2[ws_][:], in_=w2_d[l, e_].rearrange("(k p) n -> p k n", p=128)), writes=[f"w2_{ws_}"])
                    S.dma('sp', f"b2b{ws_}", lambda q: q.dma_start(
                        out=b2b[ws_][:], in_=b2_d[l, e_].partition_broadcast(128)), writes=[f"b2b{ws_}"])

                units = [(e, u) for e in range(E) for u in range(NU)]

                def load_rows(ui):
                    e_, u_ = units[ui]
                    base_ = e_ * CAPT + u_ * US
                    for b in range(ub):
                        S.dma('sp', f"rows{b}", lambda q: q.dma_start(
                            out=rows[b][:], in_=hbuf[base_ + b * 128:base_ + (b + 1) * 128, :]),
                            reads=[], writes=[f"rows{b}"])

                def transposes(ui):
                    hsl_ = ui % 2
                    for b in range(ub):
                        pb_ = (ui * ub + b) % 2
                        S.pe([lambda t, k=k: t.transpose(out=psT[pb_][:, k * 128:(k + 1) * 128],
                                                         in_=rows[b][:, k * 128:(k + 1) * 128], identity=ident)
                              for k in range(8)], reads=[f"rows{b}"], writes=[f"psTb{pb_}"])
                        if b % 2:
                            S.op('act', lambda a: a.copy(out=hTg[hsl_][:, :, b * 128:(b + 1) * 128],
                                                         in_=psT[pb_][:].rearrange("p (k t) -> p k t", t=128)),
                                 reads=[f"psTb{pb_}"], writes=[f"hTg{hsl_}_{b}"])
                        else:
                            S.op('dve', lambda v: v.tensor_copy(out=hTg[hsl_][:, :, b * 128:(b + 1) * 128],
                                                                in_=psT[pb_][:].rearrange("p (k t) -> p k t", t=128)),
                                 reads=[f"psTb{pb_}"], writes=[f"hTg{hsl_}_{b}"])

                load_w(0)
                load_rows(0)
                transposes(0)
                for ui, (e, u) in enumerate(units):
                    ws = e % 2
                    hsl = ui % 2
                    base = e * CAPT + u * US
                    hkeys = [f"hTg{hsl}_{b}" for b in range(ub)]
                    if u == 0 and e + 1 < E:
                        load_w(e + 1)
                    if ui + 1 < len(units):
                        load_rows(ui + 1)

                    def fin(fc_):
                        t2 = fc_ % 2
                        S.op('dve', lambda v: v.scalar_tensor_tensor(
                            out=actT[:, fc_, :].rearrange("p (a b) -> p a b", b=NW), in0=au[t2][:], scalar=-6.0,
                            in1=tg[t2][:], op0=ALU.max, op1=ALU.mult),
                            reads=[f"au{t2}", f"tg{t2}"], writes=[f"actT{fc_}"])

                    for fc in range(8):
                        ts2 = fc % 2
                        for (pst, col0, nm) in ((psG, fc * 128, 'psG'), (psU, D + fc * 128, 'psU')):
                            for ns in range(NSPL):
                                S.pe([lambda t, k=k: t.matmul(pst[:, ns, 0:NW], w1[ws][:, k, col0:col0 + 128],
                                                              hTg[hsl][:, k, ns * NW:(ns + 1) * NW],
                                                              start=(k == 0), stop=(k == 7)) for k in range(8)],
                                     reads=[f"w1_{ws}"] + hkeys, writes=[f"{nm}{ns}"])
                        for ns in range(NSPL):
                            S.op('dve', lambda v: v.tensor_scalar(
                                out=g2[ts2][:, ns, :], in0=psG[:, ns, 0:NW],
                                scalar1=b1[:, e * 16 + fc:e * 16 + fc + 1],
                                scalar2=7.0, op0=ALU.add, op1=ALU.min),
                                reads=[f"psG{ns}", 'ppar'], writes=[f"g2_{ts2}_{ns}"])
                        for ns in range(NSPL):
                            S.op('dve', lambda v: v.tensor_scalar(
                                out=au[ts2][:, ns, :], in0=psU[:, ns, 0:NW], scalar1=b1p1[:, e, fc:fc + 1],
                                scalar2=8.0, op0=ALU.add, op1=ALU.min),
                                reads=[f"psU{ns}", 'b1p1'], writes=[f"au{ts2}"])
                        S.op('act', lambda a: a.activation(out=sgx[ts2][:], in_=g2[ts2][:], func=AF.Sigmoid,
                                                           scale=1.702),
                             reads=[f"g2_{ts2}_{ns}" for ns in range(NSPL)], writes=[f"sgx{ts2}"])
                        S.op('pool', lambda g_: g_.tensor_tensor(out=tg[ts2][:], in0=g2[ts2][:], in1=sgx[ts2][:],
                                                                 op=ALU.mult),
                             reads=[f"g2_{ts2}_{ns}" for ns in range(NSPL)] + [f"sgx{ts2}"], writes=[f"tg{ts2}"])
                        if fc > 0:
                            fin(fc - 1)
                    fin(7)
                    if ui + 1 < len(units):
                        transposes(ui + 1)
                    akeys = [f"actT{fc}" for fc in range(8)]
                    for b in range(ub):
                        ysl = b % 2
                        for hf in range(2):
                            S.pe([lambda t, fc=fc: t.matmul(psY[hf][:], actT[:, fc, b * 128:(b + 1) * 128],
                                                            w2[ws][:, fc, hf * 512:(hf + 1) * 512],
                                                            start=(fc == 0), stop=(fc == 7)) for fc in range(8)],
                                 reads=akeys + [f"w2_{ws}"], writes=[f"psY{hf}"])
                            S.op('dve', lambda v: v.tensor_tensor(out=yst[ysl][:, hf * 512:(hf + 1) * 512],
                                                                  in0=psY[hf][:],
                                                                  in1=b2b[ws][:, hf * 512:(hf + 1) * 512], op=ALU.add),
                                 reads=[f"psY{hf}", f"b2b{ws}"], writes=[f"yst{ysl}_{hf}"])
                        S.dma('sp', f"yst{ysl}", lambda q: q.dma_start(
                            out=ybuf[base + b * 128:base + (b + 1) * 128, :], in_=yst[ysl][:]),
                            reads=[f"yst{ysl}_0", f"yst{ysl}_1"], writes=[])
                S.barrier()

            with contextlib.ExitStack() as pd:
                lnbc = sb("lnbc2", [128, 2, D], F32, pd)
                S.dma('sp', 'par_ln', lambda q: q.dma_start(
                    out=lnbc[:].rearrange("p a d -> p (a d)"),
                    in_=lnv_d[l, 2:4].rearrange("a d -> (a d)").partition_broadcast(128)), writes=['lnbc'])
                NS_D = 3
                yk = [sb(f"yk{i}", [128, 4, D], BF16, pd) for i in range(NS_D)]
                acc = [sb(f"acc{i}", [128, D], F32, pd) for i in range(NS_D)]
                ot = [sb(f"ot{i}", [128, D], F32, pd) for i in range(NS_D)]
                st6d = sb("st6d", [128, 2, 6], F32, pd)
                mvd = sb("mvd", [128, 2], F32, pd)
                rsd = sb("rsd", [128, 1], F32, pd)
                nbd = sb("nbd", [128, 1], F32, pd)
                dgG = sb("dgG", [128, 4, 128], BF16, pd)
                psD = [ps(f"psD{i}", [128, 512], F32, pd) for i in range(2)]

                def load_d(t_):
                    sl_ = t_ % NS_D
                    for k in range(4):
                        S.dma('pool', f"yk{sl_}", lambda q: q.indirect_dma_start(
                            out=yk[sl_][:, k, :], out_offset=None, in_=ybuf,
                            in_offset=bass.IndirectOffsetOnAxis(ap=dest_all[:, t_, k:k + 1], axis=0)),
                            reads=[], writes=[f"yk{sl_}_{k}"])
                    S.dma('sp', f"acc{sl_}", lambda q: q.dma_start(out=acc[sl_][:],
                                                                   in_=r_buf[t_ * 128:(t_ + 1) * 128, :]),
                          reads=[], writes=[f"acc{sl_}"])

                for t_ in range(min(NS_D - 1, NT128)):
                    load_d(t_)
                for t in range(NT128):
                    sl = t % NS_D
                    if t + NS_D - 1 < NT128:
                        load_d(t + NS_D - 1)
                    for k in range(4):
                        S.op('dve', lambda v: v.tensor_scalar(out=dgG[:, k, :], in0=ident_f,
                                                              scalar1=gate_all[:, t, k:k + 1], scalar2=None,
                                                              op0=ALU.mult), reads=[], writes=[f"dgG{k}"])
                    for hf in range(2):
                        S.pe([lambda t_, k=k: t_.matmul(psD[hf][:], dgG[:, k, :], yk[sl][:, k, hf * 512:(hf + 1) * 512],
                                                        start=(k == 0), stop=(k == 3)) for k in range(4)],
                             reads=[f"yk{sl}_{kk}" for kk in range(4)] + [f"dgG{kk}" for kk in range(4)],
                             writes=[f"psD{hf}"])
                        S.op('dve', lambda v: v.tensor_tensor(out=acc[sl][:, hf * 512:(hf + 1) * 512],
                                                              in0=psD[hf][:], in1=acc[sl][:, hf * 512:(hf + 1) * 512],
                                                              op=ALU.add),
                             reads=[f"psD{hf}", f"acc{sl}"], writes=[f"acc{sl}"])
                    for hf in range(2):
                        S.op('dve', lambda v: v.bn_stats(out=st6d[:, hf, :], in_=acc[sl][:, hf * 512:(hf + 1) * 512]),
                             reads=[f"acc{sl}"], writes=['st6d'])
                    S.op('dve', lambda v: v.bn_aggr(out=mvd[:], in_=st6d[:].rearrange("p a b -> p (a b)")),
                         reads=['st6d'], writes=['mvd'])
                    S.op('act', lambda a: a.activation(out=rsd[:], in_=mvd[:, 1:2], func=AF.Sqrt, bias=epsc[:, 0:1]),
                         reads=['mvd'], writes=['rsd'])
                    S.op('dve', lambda v: v.reciprocal(out=rsd[:], in_=rsd[:]), reads=['rsd'], writes=['rsd'])
                    S.op('dve', lambda v: v.scalar_tensor_tensor(out=nbd[:], in0=mvd[:, 0:1], scalar=-1.0, in1=rsd[:],
                                                                 op0=ALU.mult, op1=ALU.mult),
                         reads=['mvd', 'rsd'], writes=['nbd'])
                    S.op('act', lambda a: a.activation(out=ot[sl][:], in_=acc[sl][:], func=AF.Identity,
                                                       scale=rsd[:, 0:1], bias=nbd[:, 0:1]),
                         reads=[f"acc{sl}", 'rsd', 'nbd'], writes=[f"ot{sl}"])
                    S.op('dve', lambda v: v.tensor_tensor(out=ot[sl][:], in0=ot[sl][:], in1=lnbc[:, 0, :],
                                                          op=ALU.mult), reads=[f"ot{sl}", 'lnbc'], writes=[f"ot{sl}"])
                    S.op('dve', lambda v: v.tensor_tensor(out=ot[sl][:], in0=ot[sl][:], in1=lnbc[:, 1, :],
                                                          op=ALU.add), reads=[f"ot{sl}", 'lnbc'], writes=[f"ot{sl}"])
                    S.dma('sp', f"ot{sl}", lambda q: q.dma_start(out=dst_x[t * 128:(t + 1) * 128, :], in_=ot[sl][:]),
                          reads=[f"ot{sl}"], writes=[])
                S.barrier()
    return nc


def _consts(capb):
    CAPT = capb * 128
    TRASH1 = E * CAPT + 1
    cf = np.zeros((128, 192), np.float32)
    cf[:, 0:128] = np.eye(128, dtype=np.float32)
    cf[:, 128:160] = (np.arange(E, dtype=np.float32) * CAPT + 1 - TRASH1)[None, :]
    wins = (2, 4, 8, 16)
    for j in range(2):
        for p in range(128):
            w = wins[j * 2 + p // 64]
            for t in range(16):
                cf[p, 160 + j * 16 + t] = w / min(t + 1, w)
    cb = np.zeros((128, 3 * 128 + 4096), np.float32)
    cb[:, 0:128] = np.eye(128)
    cb[:, 128:256] = np.triu(np.ones((128, 128)), 1)
    cb[:, 256:384] = 1.0
    pd = np.zeros((128, 2, 16, 128), np.float32)
    for j in range(2):
        for p in range(128):
            w = wins[j * 2 + p // 64]
            for k in range(w):
                pd[p, j, k, p] = 1.0 / w
    cb[:, 384:] = pd.reshape(128, 4096)
    return cf, cb.astype(ml_dtypes.bfloat16)


def _pack_pp(conv_a_w, conv_a_b, ln_a_g, ln_a_b, conv_b_w, pool_scale, b_gate_up):
    L = conv_a_w.shape[0]
    chan = lambda a, nj: a.reshape(L, nj, 128).transpose(0, 2, 1)
    caw = conv_a_w.transpose(0, 2, 1).reshape(L, 3, 128, 31).transpose(0, 2, 1, 3).reshape(L, 128, 93)
    cbw = conv_b_w.transpose(0, 2, 1).reshape(L, 3, 128, 3).transpose(0, 2, 1, 3).reshape(L, 128, 9)
    b1 = b_gate_up.reshape(L, E, 16, 128).transpose(0, 3, 1, 2).reshape(L, 128, 512)
    return np.ascontiguousarray(np.concatenate(
        [caw, chan(conv_a_b, 3), chan(ln_a_g, 3), chan(ln_a_b, 3), cbw, chan(pool_scale, 2), b1], axis=2),
        dtype=np.float32)


def run(inputs, nseq, depth, capb, ub, ncores=NCORES):
    f = lambda k: np.asarray(inputs[k], dtype=np.float32)
    x, p = f("x"), f("p")
    nc = build_program(nseq, depth, capb, ub)
    cf, cb = _consts(capb)
    pp = _pack_pp(f("conv_a_w")[:depth], f("conv_a_b")[:depth], f("ln_a_g")[:depth], f("ln_a_b")[:depth],
                  f("conv_b_w")[:depth], f("pool_scale")[:depth], f("b_gate_up")[:depth])
    lnv = np.ascontiguousarray(np.stack([f("ln1_g")[:depth], f("ln1_b")[:depth], f("ln2_g")[:depth],
                                         f("ln2_b")[:depth]], axis=1))
    shared = {
        "w_in": f("w_in")[:depth], "w_out": f("w_out")[:depth], "ple_w_gate": f("ple_w_gate")[:depth],
        "ple_w_proj": f("ple_w_proj")[:depth], "router_w": f("router_w")[:depth], "router_b": f("router_b")[:depth],
        "w_gate_up": f("w_gate_up")[:depth], "w_down": f("w_down")[:depth], "b_down": f("b_down")[:depth],
        "pool_w": f("pool_w")[:depth], "lnv": lnv, "pp": pp, "cst_f": cf, "cst_b": cb,
    }
    in_maps = []
    for c in range(ncores):
        m = dict(shared)
        m["x"] = x[c * nseq:(c + 1) * nseq].reshape(nseq * SEQ, D)
        m["p"] = p[:depth, c * nseq:(c + 1) * nseq].reshape(depth, nseq * SEQ, PLE)
        in_maps.append(m)
    res = run_bass_kernel_spmd(nc, in_maps, core_ids=list(range(ncores)))
    out = np.stack([r["out"].reshape(nseq, SEQ, D) for r in res.results], axis=0)
    return out.reshape(ncores * nseq, SEQ, D).astype(np.float32)


def kernel(**inputs):
    return run(inputs, nseq=4, depth=4, capb=10, ub=5)
```

```python
import contextlib
import numpy as np
import ml_dtypes
import concourse.bass as bass
import concourse.mybir as mybir
from concourse.bass_utils import run_bass_kernel_spmd

F32 = mybir.dt.float32
BF16 = mybir.dt.bfloat16
I32 = mybir.dt.int32
ALU = mybir.AluOpType
AF = mybir.ActivationFunctionType
AX = mybir.AxisListType

D = 1024
DIN = 2176
NCH = 17
E = 32
PLE = 256
TT = 256
SEQ = 2048
LN_EPS = 1e-5
NCORES = 8


class Sched:
    def __init__(self, nc, es):
        self.nc = nc
        self.es = es
        self.eng = {'pe': nc.tensor, 'dve': nc.vector, 'act': nc.scalar, 'pool': nc.gpsimd, 'sp': nc.sync}
        self.sem = {e: es.enter_context(nc.semaphore(f"sem_{e}")) for e in ('pe', 'dve', 'act', 'pool')}
        self.cnt = {e: 0 for e in ('pe', 'dve', 'act', 'pool')}
        self.dsem = {}
        self.dcnt = {}
        self.last_w = {}
        self.readers = {}
        self.waited = {}
        self.nwait = 0

    def _wait(self, eng, tok):
        kind, who, val = tok
        if kind == 'c' and who == 'pe' and eng == 'pe':
            return
        key = (eng, kind, who)
        if self.waited.get(key, 0) >= val:
            return
        semh = self.sem[who] if kind == 'c' else self.dsem[who]
        self.eng[eng].wait_ge(semh, val)
        self.waited[key] = val
        self.nwait += 1

    def _deps(self, eng, reads, writes):
        for k in reads:
            t = self.last_w.get(k)
            if t is not None:
                self._wait(eng, t)
        for k in writes:
            t = self.last_w.get(k)
            if t is not None:
                self._wait(eng, t)
            for (kind, who), val in self.readers.get(k, {}).items():
                self._wait(eng, (kind, who, val))

    def _commit(self, tok, reads, writes):
        kind, who, val = tok
        for k in reads:
            self.readers.setdefault(k, {})[(kind, who)] = val
        for k in writes:
            self.last_w[k] = tok
            self.readers[k] = {}

    def op(self, eng, fn, reads=(), writes=()):
        self._deps(eng, reads, writes)
        ins = fn(self.eng[eng])
        self.cnt[eng] += 1
        ins.then_inc(self.sem[eng], 1)
        self._commit(('c', eng, self.cnt[eng]), reads, writes)

    def pe(self, fns, reads=(), writes=()):
        self._deps('pe', reads, writes)
        ins = None
        for f in fns:
            ins = f(self.nc.tensor)
        self.cnt['pe'] += 1
        ins.then_inc(self.sem['pe'], 1)
        self._commit(('c', 'pe', self.cnt['pe']), reads, writes)

    def dma(self, q, group, fn, reads=(), writes=()):
        if group not in self.dsem:
            self.dsem[group] = self.es.enter_context(self.nc.semaphore(f"dsem_{group}"))
            self.dcnt[group] = 0
        self._deps(q, reads, writes)
        ins = fn(self.eng[q])
        self.dcnt[group] += 16
        ins.then_inc(self.dsem[group], 16)
        self._commit(('d', group, self.dcnt[group]), reads, writes)

    def group_sync(self, group, keys):
        tok = ('d', group, self.dcnt[group])
        for k in keys:
            self.last_w[k] = tok
            self.readers[k] = {}

    def barrier(self):
        toks = [('c', e, self.cnt[e]) for e in self.cnt if self.cnt[e] > 0]
        toks += [('d', g, self.dcnt[g]) for g in self.dcnt if self.dcnt[g] > 0]
        for e in ('pe', 'dve', 'act', 'pool', 'sp'):
            for t in toks:
                self._wait(e, t)
        self.last_w = {}
        self.readers = {}


def build_program(nseq, depth, capb, ub):
    NTOK = nseq * SEQ
    NT = NTOK // TT
    TPS = SEQ // TT
    NT128 = NTOK // 128
    CAPT = capb * 128
    NSLOT = E * CAPT + 128
    TRASH = E * CAPT
    NU = capb // ub
    US = ub * 128
    NSPL = (US + 511) // 512
    NW = US // NSPL
    assert NW * NSPL == US and capb % ub == 0
    ALPHA = float((2 * 4) ** 0.25)

    nc = bass.Bass("TRN2", target_bir_lowering=False)
    dt = lambda name, shape, dtype, kind: nc.dram_tensor(name, shape, dtype, kind=kind).ap()
    x_in = dt("x", [NTOK, D], F32, "ExternalInput")
    p_in = dt("p", [depth, NTOK, PLE], F32, "ExternalInput")
    w_in_d = dt("w_in", [depth, D, DIN], F32, "ExternalInput")
    w_out_d = dt("w_out", [depth, D, D], F32, "ExternalInput")
    pg_d = dt("ple_w_gate", [depth, D, D], F32, "ExternalInput")
    ppj_d = dt("ple_w_proj", [depth, PLE, D], F32, "ExternalInput")
    rw_d = dt("router_w", [depth, D, E], F32, "ExternalInput")
    rb_d = dt("router_b", [depth, E], F32, "ExternalInput")
    w1_d = dt("w_gate_up", [depth, E, D, 2 * D], F32, "ExternalInput")
    w2_d = dt("w_down", [depth, E, D, D], F32, "ExternalInput")
    b2_d = dt("b_down", [depth, E, D], F32, "ExternalInput")
    poolw_d = dt("pool_w", [depth, 4, 64, 64], F32, "ExternalInput")
    lnv_d = dt("lnv", [depth, 4, D], F32, "ExternalInput")
    NPP = 93 + 3 + 3 + 3 + 9 + 2 + 512
    pp_d = dt("pp", [depth, 128, NPP], F32, "ExternalInput")
    cst_f_d = dt("cst_f", [128, 128 + 32 + 32], F32, "ExternalInput")
    cst_b_d = dt("cst_b", [128, 3 * 128 + 2 * 16 * 128], BF16, "ExternalInput")
    out_d = dt("out", [NTOK, D], F32, "ExternalOutput")
    r_buf = dt("r_buf", [NTOK, D], F32, "Internal")
    hbuf = dt("hbuf", [NSLOT, D], BF16, "Internal")
    ybuf = dt("ybuf", [NSLOT, D], BF16, "Internal")

    es = contextlib.ExitStack()
    with es:
        S = Sched(nc, es)
        uid = [0]

        def sb(name, shape, dtype, stack=es):
            uid[0] += 1
            return stack.enter_context(nc.sbuf_tensor(f"s{uid[0]}_{name}", shape, dtype))

        def ps(name, shape, dtype, stack=es):
            uid[0] += 1
            return stack.enter_context(nc.psum_tensor(f"q{uid[0]}_{name}", shape, dtype))

        cst_f = sb("cst_f", [128, 192], F32)
        cst_b = sb("cst_b", [128, 3 * 128 + 2 * 16 * 128], BF16)
        ident_f = cst_f[:, 0:128]
        ebase1m = cst_f[:, 128:160]
        poolcorr = cst_f[:, 160:192]
        ident = cst_b[:, 0:128]
        ltri = cst_b[:, 128:256]
        ones = cst_b[:, 256:384]
        pooldg = cst_b[:, 384:384 + 4096]
        dest_all = sb("dest_all", [128, NT128, 4], I32)
        gate_all = sb("gate_all", [128, NT128, 4], F32)
        run_cnt = sb("run_cnt", [128, E], F32)
        epsc = sb("epsc", [128, 1], F32)
        S.op('dve', lambda v: v.memset(epsc[:], LN_EPS), writes=['epsc'])
        ppar = sb("ppar", [128, NPP], F32)
        b1p1 = sb("b1p1", [128, E, 8], F32)

        S.dma('sp', 'cst', lambda q: q.dma_start(out=cst_f[:], in_=cst_f_d), writes=['cst'])
        S.dma('sp', 'cst', lambda q: q.dma_start(out=cst_b[:], in_=cst_b_d), writes=['cst'])
        with contextlib.ExitStack() as p0:
            zrow = sb("zrow", [128, D], BF16, p0)
            S.op('dve', lambda v: v.memset(zrow[:], 0.0), writes=['zrow'])
            S.dma('sp', 'cst', lambda q: q.dma_start(out=ybuf[TRASH:TRASH + 128, :], in_=zrow[:]),
                  reads=['zrow'], writes=['ybuf_trash'])
            S.barrier()

        caw = ppar[:, 0:93]
        cab = ppar[:, 93:96]
        lag = ppar[:, 96:99]
        lab = ppar[:, 99:102]
        cbw = ppar[:, 102:111]
        psc = ppar[:, 111:113]
        b1 = ppar[:, 113:113 + 512]

        for l in range(depth):
            src_x = x_in if l == 0 else r_buf
            dst_x = out_d if l == depth - 1 else r_buf
            S.dma('sp', 'par_pp', lambda q: q.dma_start(out=ppar[:], in_=pp_d[l]), writes=['ppar'])
            S.op('dve', lambda v: v.tensor_scalar(
                out=b1p1[:], in0=b1.rearrange("p (e c) -> p e c", c=16)[:, :, 8:16], scalar1=1.0, scalar2=None,
                op0=ALU.add), reads=['ppar'], writes=['b1p1'])
            S.op('dve', lambda v: v.memset(run_cnt[:], 0.0), writes=['run_cnt'])

            with contextlib.ExitStack() as pa:
                lnbc = sb("lnbc1", [128, 2, D], F32, pa)
                S.dma('sp', 'par_ln', lambda q: q.dma_start(
                    out=lnbc[:].rearrange("p a d -> p (a d)"),
                    in_=lnv_d[l, 0:2].rearrange("a d -> (a d)").partition_broadcast(128)), writes=['lnbc'])
                w_in = sb("w_in", [128, 8, DIN], BF16, pa)
                w_out = sb("w_out", [128, 8, D], BF16, pa)
                pgw = sb("pgw", [128, 8, D], BF16, pa)
                ppw = sb("ppw", [128, 2, D], BF16, pa)
                rw = sb("rw", [128, 8, E], BF16, pa)
                rbb = sb("rbb", [128, E], F32, pa)
                dgA = sb("dgA", [128, 93, 128], BF16, pa)
                pwbd = sb("pwbd", [128, 2, 128], BF16, pa)
                x_tm = [sb("x_tm0", [128, 2, D], F32, pa)]
                xres = [sb(f"xres{i}", [128, D], F32, pa) for i in range(2)]
                p_tm = [sb("p_tm0", [128, 2, PLE], F32, pa)] * 2
                xb = sb("xb", [128, 2, D], BF16, pa)
                xT = sb("xT", [128, 8, TT], BF16, pa)
                sg = sb("sg", [128, TT], F32, pa)
                uh = sb("uh", [128, 3, 30 + TT], BF16, pa)
                wh = sb("wh", [128, 3, 2 + TT], F32, pa)
                bgt = sb("bgt", [128, 1, TT], F32, pa)
                cgt = sg
                tb = sb("tb", [128, TT], F32, pa)
                ph = sb("ph", [128, 2, 15 + TT], BF16, pa)
                pooled = sb("pooled", [128, 2, TT], BF16, pa)
                v32 = sb("v32", [128, 3, TT], F32, pa)
                vb = sb("vb", [128, 3, TT], BF16, pa)
                vsq = sb("vsq", [128, 3, TT], BF16, pa)
                mean_sb = sb("mean_sb", [128, TT], F32, pa)
                var_sb = sb("var_sb", [128, TT], F32, pa)
                rstd = sb("rstd", [128, TT], F32, pa)
                tn = tb
                yT = [sb(f"yT{i}", [128, 8, TT], BF16, pa) for i in range(2)]
                sbuf_s = sb("s_s", [128, D], F32, pa)
                h32 = sb("h32", [128, D], F32, pa)
                hb = [sb(f"hb{i}", [128, D], BF16, pa) for i in range(2)]
                hT = sb("hT", [128, 8, TT], BF16, pa)
                pb = sb("pb", [128, 2, PLE], BF16, pa)
                pT = sb("pT", [128, 2, TT], BF16, pa)
                sgm = sbuf_s
                rt = [sb("rt0", [128, D], F32, pa)] * 2
                st6 = sb("st6", [128, 2, 6], F32, pa)
                mv = sb("mv", [128, 2], F32, pa)
                rs1 = sb("rs1", [128, 1], F32, pa)
                lg = sb("lg", [128, E], F32, pa)
                mx8 = sb("mx8", [128, 8], F32, pa)
                msk = sb("msk", [128, E], F32, pa)
                mskb = sb("mskb", [128, E], BF16, pa)
                nmx = sb("nmx", [128, 1], F32, pa)
                ex = sb("ex", [128, E], F32, pa)
                ssum = sb("ssum", [128, 1], F32, pa)
                G = sb("G", [128, E], F32, pa)
                Cr = sb("Cr", [128, E], F32, pa)
                okm = sb("okm", [128, E], F32, pa)
                Vv = sb("Vv", [128, E], F32, pa)
                d8 = sb("d8", [128, 8], F32, pa)
                eqg = sb("eqg", [128, E], F32, pa)
                psT = [ps(f"psT{i}", [128, D], BF16, pa) for i in range(2)]
                psA = [ps(f"psA{i}", [128, 512], F32, pa) for i in range(4)]
                psB = [ps(f"psB{i}", [128, 512], F32, pa) for i in range(2)]

                for hlf in range(2):
                    S.dma('pool', 'wA', lambda q: q.dma_start(
                        out=w_in[:, :, hlf * 1088:(hlf + 1) * 1088],
                        in_=w_in_d[l].rearrange("(k p) n -> p k n", p=128)[:, :, hlf * 1088:(hlf + 1) * 1088]),
                        writes=['w_in'])
                S.dma('pool', 'wA', lambda q: q.dma_start(
                    out=w_out[:], in_=w_out_d[l].rearrange("(k p) n -> p k n", p=128)), writes=['w_out'])
                S.dma('pool', 'wA', lambda q: q.dma_start(
                    out=pgw[:], in_=pg_d[l].rearrange("(k p) n -> p k n", p=128)), writes=['pgw'])
                S.dma('pool', 'wA', lambda q: q.dma_start(
                    out=ppw[:], in_=ppj_d[l].rearrange("(k p) n -> p k n", p=128)), writes=['ppw'])
                S.dma('pool', 'wA', lambda q: q.dma_start(
                    out=rw[:], in_=rw_d[l].rearrange("(k p) n -> p k n", p=128)), writes=['rw'])
                S.dma('sp', 'par_rb', lambda q: q.dma_start(out=rbb[:], in_=rb_d[l].partition_broadcast(128)),
                      writes=['rbb'])
                S.op('dve', lambda v: v.memset(pwbd[:], 0.0), writes=['pwbd'])
                for g in range(4):
                    j, o = g // 2, (g % 2) * 64
                    S.dma('pool', 'wA', lambda q: q.dma_start(out=pwbd[o:o + 64, j, o:o + 64], in_=poolw_d[l, g]),
                          reads=[], writes=['pwbd'])
                S.group_sync('wA', ['w_in', 'w_out', 'pgw', 'ppw', 'rw', 'pwbd'])
                for jk in range(93):
                    S.op('dve', lambda v: v.tensor_scalar(out=dgA[:, jk, :], in0=ident_f, scalar1=caw[:, jk:jk + 1],
                                                          scalar2=None, op0=ALU.mult),
                         reads=['ppar', 'cst'], writes=['dgA'])

                def load_x(it_):
                    S.dma('sp', "x_tm0", lambda q: q.dma_start(
                        out=x_tm[0][:],
                        in_=src_x[it_ * TT:(it_ + 1) * TT, :].rearrange("(s p) d -> p s d", p=128)),
                        reads=[], writes=["x_tm0"])

                def zmm(c, bank):
                    S.pe([lambda t, k=k: t.matmul(psA[bank][:, 0:TT], w_in[:, k, c * 128:(c + 1) * 128],
                                                  xT[:, k, :], start=(k == 0), stop=(k == 7))
                          for k in range(8)], reads=['w_in', 'xT'], writes=[f"psA{bank}"])

                def front_steps(it):
                    ts_ = it % TPS
                    ysl = it % 2
                    yTc = yT[ysl]
                    kyT = f"yT{ysl}"
                    steps = {}

                    def f_xc():
                        S.op('act', lambda a: a.copy(out=xb[:], in_=x_tm[0][:]), reads=["x_tm0"], writes=['xb'])
                        if it + 1 < NT:
                            load_x(it + 1)
                    steps['XC'] = f_xc

                    def f_xT():
                        for s in range(2):
                            S.pe([lambda t, k=k: t.transpose(out=psT[s][:, k * 128:(k + 1) * 128],
                                                             in_=xb[:, s, k * 128:(k + 1) * 128], identity=ident)
                                  for k in range(8)], reads=['xb'], writes=[f"psT{s}"])
                            S.op('dve', lambda v: v.tensor_copy(out=xT[:, :, s * 128:(s + 1) * 128],
                                                                in_=psT[s][:].rearrange("p (k t) -> p k t", t=128)),
                                 reads=[f"psT{s}"], writes=['xT'])
                        if ts_ == 0:
                            S.op('pool', lambda g_: g_.memset(uh[:, :, 0:30], 0.0), writes=['uh0', 'uh1', 'uh2'])
                            S.op('pool', lambda g_: g_.memset(wh[:, :, 0:2], 0.0), writes=['wh0', 'wh1', 'wh2'])
                            S.op('pool', lambda g_: g_.memset(ph[:, :, 0:15], 0.0), writes=['ph0', 'ph1'])
                        else:
                            S.op('pool', lambda g_: g_.tensor_copy(out=uh[:, :, 0:30], in_=uh[:, :, TT:TT + 30]),
                                 reads=['uh0', 'uh1', 'uh2'], writes=['uh0', 'uh1', 'uh2'])
                            S.op('pool', lambda g_: g_.tensor_copy(out=wh[:, :, 0:2], in_=wh[:, :, TT:TT + 2]),
                                 reads=['wh0', 'wh1', 'wh2'], writes=['wh0', 'wh1', 'wh2'])
                            S.op('pool', lambda g_: g_.tensor_copy(out=ph[:, :, 0:15], in_=ph[:, :, TT:TT + 15]),
                                 reads=['ph0', 'ph1'], writes=['ph0', 'ph1'])
                    steps['X'] = f_xT

                    def f_A(j):
                        def f():
                            zmm(3 + j, 0)
                            S.op('act', lambda a: a.activation(out=sg[:], in_=psA[0][:, 0:TT], func=AF.Sigmoid),
                                 reads=['psA0'], writes=['sg'])
                            zmm(j, 1)
                            S.op('dve', lambda v: v.tensor_tensor(out=uh[:, j, 30:30 + TT], in0=psA[1][:, 0:TT],
                                                                  in1=sg[:], op=ALU.mult),
                                 reads=['psA1', 'sg'], writes=[f"uh{j}"])
                        return f
                    for j in range(3):
                        steps[f'A{j}'] = f_A(j)

                    def f_conv(j):
                        def f():
                            bank = j % 2
                            S.pe([lambda t, k=k: t.matmul(psA[bank][:, 0:TT], dgA[:, j * 31 + k, :],
                                                          uh[:, j, k:k + TT], start=(k == 0), stop=(k == 30))
                                  for k in range(31)], reads=['dgA', f"uh{j}"], writes=[f"psA{bank}"])
                            S.op('act', lambda a: a.activation(out=v32[:, j, :], in_=psA[bank][:, 0:TT],
                                                               func=AF.Identity, bias=cab[:, j:j + 1]),
                                 reads=[f"psA{bank}", 'ppar'], writes=[f"v32_{j}"])
                            S.op('act', lambda a: a.activation(out=vsq[:, j, :], in_=psA[bank][:, 0:TT],
                                                               func=AF.Square, bias=cab[:, j:j + 1]),
                                 reads=[f"psA{bank}", 'ppar'], writes=[f"vsq{j}"])
                            S.op('pool', lambda g_: g_.tensor_copy(out=vb[:, j, :], in_=v32[:, j, :]),
                                 reads=[f"v32_{j}"], writes=[f"vb{j}"])
                        return f

                    def f_B(j):
                        def f():
                            zmm(6 + j, 2)
                            S.op('act', lambda a: a.copy(out=bgt[:, 0, :], in_=psA[2][:, 0:TT]),
                                 reads=['psA2'], writes=['bgt'])
                            zmm(9 + j, 3)
                            S.op('act', lambda a: a.copy(out=cgt[:], in_=psA[3][:, 0:TT]),
                                 reads=['psA3'], writes=['sg'])
                            zmm(12 + j, 2)
                            S.op('dve', lambda v: v.tensor_tensor(out=wh[:, j, 2:2 + TT], in0=psA[2][:, 0:TT],
                                                                  in1=cgt[:], op=ALU.mult),
                                 reads=['psA2', 'sg'], writes=[f"wh{j}"])
                            S.op('dve', lambda v: v.tensor_scalar(out=tb[:], in0=wh[:, j, 2:2 + TT],
                                                                  scalar1=cbw[:, j * 3 + 2:j * 3 + 3], scalar2=None,
                                                                  op0=ALU.mult), reads=[f"wh{j}", 'ppar'], writes=['tb'])
                            for k in (1, 0):
                                S.op('dve', lambda v: v.scalar_tensor_tensor(
                                    out=tb[:], in0=wh[:, j, k:k + TT], scalar=cbw[:, j * 3 + k:j * 3 + k + 1],
                                    in1=tb[:], op0=ALU.mult, op1=ALU.add), reads=[f"wh{j}", 'tb', 'ppar'], writes=['tb'])
                            S.op('dve', lambda v: v.tensor_tensor(out=yTc[:, 3 + j, :], in0=tb[:], in1=bgt[:, 0, :],
                                                                  op=ALU.mult), reads=['tb', 'bgt'], writes=[f"{kyT}_{3 + j}"])
                        return f
                    for j in range(3):
                        steps[f'C{j}'] = f_conv(j)
                        steps[f'B{j}'] = f_B(j)

                    def f_pool():
                        for j in range(2):
                            zmm(15 + j, 3)
                            S.op('act', lambda a: a.copy(out=ph[:, j, 15:15 + TT], in_=psA[3][:, 0:TT]),
                                 reads=['psA3'], writes=[f"ph{j}"])
                        for j in range(2):
                            ntap = 4 if j == 0 else 16
                            S.pe([lambda t, k=k: t.matmul(
                                psA[j][:, 0:TT], pooldg[:, (j * 16 + k) * 128:(j * 16 + k + 1) * 128],
                                ph[:, j, 15 - k:15 - k + TT], start=(k == 0), stop=(k == ntap - 1))
                                for k in range(ntap)], reads=[f"ph{j}", 'cst'], writes=[f"psA{j}"])
                            S.op('dve', lambda v: v.tensor_tensor(out=pooled[:, j, :], in0=psA[j][:, 0:TT],
                                                                  in1=ph[:, j, 15:15 + TT], op=ALU.subtract),
                                 reads=[f"psA{j}", f"ph{j}"], writes=[f"pooled{j}"])
                            if ts_ == 0:
                                S.op('dve', lambda v: v.tensor_tensor(out=tb[:, 0:16], in0=psA[j][:, 0:16],
                                                                      in1=poolcorr[:, j * 16:(j + 1) * 16],
                                                                      op=ALU.mult),
                                     reads=[f"psA{j}", 'cst'], writes=['tb'])
                                S.op('dve', lambda v: v.tensor_tensor(out=pooled[:, j, 0:16], in0=tb[:, 0:16],
                                                                      in1=ph[:, j, 15:31], op=ALU.subtract),
                                     reads=['tb', f"ph{j}"], writes=[f"pooled{j}"])
                        for j in range(2):
                            S.pe([lambda t: t.matmul(psA[2 + j][:, 0:TT], pwbd[:, j, :], pooled[:, j, :],
                                                     start=True, stop=True)],
                                 reads=['pwbd', f"pooled{j}"], writes=[f"psA{2 + j}"])
                            S.op('act', lambda a: a.activation(out=yTc[:, 6 + j, :], in_=psA[2 + j][:, 0:TT],
                                                               func=AF.Identity, scale=psc[:, j:j + 1]),
                                 reads=[f"psA{2 + j}", 'ppar'], writes=[f"{kyT}_{6 + j}"])
                    steps['PL'] = f_pool

                    def f_lna():
                        S.pe([lambda t, j=j: t.matmul(psA[0][:, 0:TT], ones, vb[:, j, :], start=(j == 0), stop=(j == 2))
                              for j in range(3)], reads=['vb0', 'vb1', 'vb2', 'cst'], writes=['psA0'])
                        S.pe([lambda t, j=j: t.matmul(psA[1][:, 0:TT], ones, vsq[:, j, :], start=(j == 0), stop=(j == 2))
                              for j in range(3)], reads=['vsq0', 'vsq1', 'vsq2', 'cst'], writes=['psA1'])
                        S.op('act', lambda a: a.activation(out=mean_sb[:], in_=psA[0][:, 0:TT], func=AF.Identity,
                                                           scale=1.0 / 384), reads=['psA0'], writes=['mean_sb'])
                        S.op('dve', lambda v: v.tensor_tensor(out=var_sb[:], in0=mean_sb[:], in1=mean_sb[:], op=ALU.mult),
                             reads=['mean_sb'], writes=['var_sb'])
                        S.op('dve', lambda v: v.scalar_tensor_tensor(out=var_sb[:], in0=psA[1][:, 0:TT], scalar=1.0 / 384,
                                                                     in1=var_sb[:], op0=ALU.mult, op1=ALU.subtract),
                             reads=['psA1', 'var_sb'], writes=['var_sb'])
                        S.op('act', lambda a: a.activation(out=rstd[:], in_=var_sb[:], func=AF.Sqrt, bias=epsc[:, 0:1]),
                             reads=['var_sb'], writes=['rstd'])
                        S.op('dve', lambda v: v.reciprocal(out=rstd[:], in_=rstd[:]), reads=['rstd'], writes=['rstd'])
                        for j in range(3):
                            S.op('pool', lambda g_: g_.tensor_tensor(out=tn[:], in0=v32[:, j, :], in1=mean_sb[:],
                                                                     op=ALU.subtract),
                                 reads=[f"v32_{j}", 'mean_sb'], writes=['tb'])
                            S.op('dve', lambda v: v.tensor_tensor(out=tn[:], in0=tn[:], in1=rstd[:], op=ALU.mult),
                                 reads=['tb', 'rstd'], writes=['tb'])
                            S.op('act', lambda a: a.activation(out=yTc[:, j, :], in_=tn[:], func=AF.Silu,
                                                               scale=lag[:, j:j + 1], bias=lab[:, j:j + 1]),
                                 reads=['tb', 'ppar'], writes=[f"{kyT}_{j}"])
                    steps['LN'] = f_lna
                    return steps

                def back_steps(it):
                    ysl = it % 2
                    yTc = yT[ysl]
                    kyT = f"yT{ysl}"
                    tok0 = it * TT

                    def b_prefetch():
                        S.dma('sp', "p_tm0", lambda q: q.dma_start(
                            out=p_tm[0][:], in_=p_in[l, tok0:tok0 + TT, :].rearrange("(s p) d -> p s d", p=128)),
                            writes=["p_tm0"])
                        for s in range(2):
                            S.dma('sp', f"xres{s}", lambda q: q.dma_start(
                                out=xres[s][:], in_=src_x[tok0 + s * 128:tok0 + (s + 1) * 128, :]),
                                writes=[f"xres{s}"])

                    def b_load():
                        S.op('act', lambda a: a.copy(out=pb[:], in_=p_tm[0][:]), reads=["p_tm0"], writes=['pb'])

                    def b_mix(s):
                        def f():
                            for hf in range(2):
                                S.pe([lambda t, k=k: t.matmul(psB[hf][:], yTc[:, k, s * 128:(s + 1) * 128],
                                                              w_out[:, k, hf * 512:(hf + 1) * 512],
                                                              start=(k == 0), stop=(k == 7))
                                      for k in range(8)], reads=[f"{kyT}_{c}" for c in range(8)] + ['w_out'], writes=[f"psB{hf}"])
                                S.op('dve', lambda v: v.scalar_tensor_tensor(
                                    out=sbuf_s[:, hf * 512:(hf + 1) * 512], in0=xres[s][:, hf * 512:(hf + 1) * 512],
                                    scalar=ALPHA, in1=psB[hf][:], op0=ALU.mult, op1=ALU.add),
                                    reads=[f"xres{s}", f"psB{hf}"], writes=['s_s'])
                                S.op('dve', lambda v: v.bn_stats(out=st6[:, hf, :],
                                                                 in_=sbuf_s[:, hf * 512:(hf + 1) * 512]),
                                     reads=['s_s'], writes=['st6'])
                            S.op('dve', lambda v: v.bn_aggr(out=mv[:], in_=st6[:].rearrange("p a b -> p (a b)")),
                                 reads=['st6'], writes=['mv'])
                        return f

                    def b_mixb(s):
                        def f():
                            S.op('act', lambda a: a.activation(out=rs1[:], in_=mv[:, 1:2], func=AF.Sqrt,
                                                               bias=epsc[:, 0:1]), reads=['mv'], writes=['rs1'])
                            S.op('dve', lambda v: v.reciprocal(out=rs1[:], in_=rs1[:]), reads=['rs1'], writes=['rs1'])
                            S.op('dve', lambda v: v.tensor_scalar(out=sbuf_s[:], in0=sbuf_s[:], scalar1=mv[:, 0:1],
                                                                  scalar2=rs1[:, 0:1], op0=ALU.subtract, op1=ALU.mult),
                                 reads=['s_s', 'mv', 'rs1'], writes=['s_s'])
                            S.op('pool', lambda g_: g_.tensor_tensor(out=sbuf_s[:], in0=sbuf_s[:], in1=lnbc[:, 0, :],
                                                                     op=ALU.mult), reads=['s_s', 'lnbc'], writes=['s_s'])
                            S.op('pool', lambda g_: g_.tensor_tensor(out=h32[:], in0=sbuf_s[:], in1=lnbc[:, 1, :],
                                                                     op=ALU.add), reads=['s_s', 'lnbc'], writes=['h32'])
                        return f

                    def b_route(s):
                        def f():
                            t128 = it * 2 + s
                            hs = t128 % 2
                            khb = f"hb{hs}"
                            S.op('act', lambda a: a.copy(out=hb[hs][:], in_=h32[:]), reads=['h32'], writes=[khb])
                            S.pe([lambda t, k=k: t.transpose(out=psT[s][:, k * 128:(k + 1) * 128],
                                                             in_=hb[hs][:, k * 128:(k + 1) * 128], identity=ident)
                                  for k in range(8)], reads=[khb], writes=[f"psT{s}"])
                            S.op('act', lambda a: a.copy(out=hT[:, :, s * 128:(s + 1) * 128],
                                                         in_=psT[s][:].rearrange("p (k t) -> p k t", t=128)),
                                 reads=[f"psT{s}"], writes=['hT'])
                        return f

                    def b_route_b(s):
                        def f():
                            t128 = it * 2 + s
                            S.pe([lambda t, k=k: t.matmul(psA[0][:, 0:E], hT[:, k, s * 128:(s + 1) * 128], rw[:, k, :],
                                                          start=(k == 0), stop=(k == 7)) for k in range(8)],
                                 reads=['hT', 'rw'], writes=['psA0'])
                            S.op('dve', lambda v: v.tensor_tensor(out=lg[:], in0=psA[0][:, 0:E], in1=rbb[:], op=ALU.add),
                                 reads=['psA0', 'rbb'], writes=['lg'])
                            S.op('dve', lambda v: v.max(out=mx8[:], in_=lg[:]), reads=['lg'], writes=['mx8'])
                            S.op('dve', lambda v: v.tensor_scalar(out=msk[:], in0=lg[:], scalar1=mx8[:, 3:4],
                                                                  scalar2=None, op0=ALU.is_ge),
                                 reads=['lg', 'mx8'], writes=['msk'])
                            S.op('dve', lambda v: v.tensor_scalar(out=nmx[:], in0=mx8[:, 0:1], scalar1=-1.0,
                                                                  scalar2=None, op0=ALU.mult),
                                 reads=['mx8'], writes=['nmx'])
                            S.op('act', lambda a: a.activation(out=ex[:], in_=lg[:], func=AF.Exp, bias=nmx[:, 0:1]),
                                 reads=['lg', 'nmx'], writes=['ex'])
                            S.op('pool', lambda g_: g_.tensor_copy(out=mskb[:], in_=msk[:]),
                                 reads=['msk'], writes=['mskb'])
                        return f

                    def b_route_c(s):
                        def f():
                            t128 = it * 2 + s
                            hs = t128 % 2
                            khb = f"hb{hs}"
                            S.pe([lambda t: t.matmul(psA[1][:, 0:E], ltri, mskb[:], start=True, stop=True)],
                                 reads=['mskb', 'cst'], writes=['psA1'])
                            S.pe([lambda t: t.matmul(psA[2][:, 0:E], ones, mskb[:], start=True, stop=True)],
                                 reads=['mskb', 'cst'], writes=['psA2'])
                            S.op('dve', lambda v: v.tensor_tensor(out=ex[:], in0=ex[:], in1=msk[:], op=ALU.mult),
                                 reads=['ex', 'msk'], writes=['ex'])
                            S.op('dve', lambda v: v.tensor_reduce(out=ssum[:], in_=ex[:], axis=AX.X, op=ALU.add),
                                 reads=['ex'], writes=['ssum'])
                            S.op('dve', lambda v: v.reciprocal(out=ssum[:], in_=ssum[:]),
                                 reads=['ssum'], writes=['ssum'])
                            S.op('dve', lambda v: v.tensor_scalar(out=G[:], in0=ex[:], scalar1=ssum[:, 0:1],
                                                                  scalar2=None, op0=ALU.mult),
                                 reads=['ex', 'ssum'], writes=['G'])
                            S.op('dve', lambda v: v.tensor_tensor(out=Cr[:], in0=psA[1][:, 0:E], in1=run_cnt[:],
                                                                  op=ALU.add), reads=['psA1', 'run_cnt'], writes=['Cr'])
                            S.op('dve', lambda v: v.tensor_tensor(out=run_cnt[:], in0=psA[2][:, 0:E], in1=run_cnt[:],
                                                                  op=ALU.add),
                                 reads=['psA2', 'run_cnt'], writes=['run_cnt'])
                            S.op('dve', lambda v: v.tensor_scalar(out=okm[:], in0=Cr[:], scalar1=float(CAPT),
                                                                  scalar2=None, op0=ALU.is_lt),
                                 reads=['Cr'], writes=['okm'])
                            S.op('dve', lambda v: v.tensor_tensor(out=Cr[:], in0=Cr[:], in1=ebase1m, op=ALU.add),
                                 reads=['Cr', 'cst'], writes=['Cr'])
                            S.op('dve', lambda v: v.tensor_tensor(out=Cr[:], in0=Cr[:], in1=okm[:], op=ALU.mult),
                                 reads=['Cr', 'okm'], writes=['Cr'])
                            S.op('dve', lambda v: v.scalar_tensor_tensor(out=Vv[:], in0=Cr[:], scalar=float(TRASH + 1),
                                                                         in1=msk[:], op0=ALU.add, op1=ALU.mult),
                                 reads=['Cr', 'msk'], writes=['Vv'])
                            S.op('dve', lambda v: v.max(out=d8[:], in_=Vv[:]), reads=['Vv'], writes=['d8'])
                            S.op('dve', lambda v: v.tensor_scalar(out=dest_all[:, t128, :], in0=d8[:, 0:4], scalar1=-1.0,
                                                                  scalar2=None, op0=ALU.add),
                                 reads=['d8'], writes=[f"dest{t128}"])
                            for k in range(4):
                                S.dma('pool', khb, lambda q: q.indirect_dma_start(
                                    out=hbuf,
                                    out_offset=bass.IndirectOffsetOnAxis(ap=dest_all[:, t128, k:k + 1], axis=0),
                                    in_=hb[hs][:], in_offset=None), reads=[khb, f"dest{t128}"], writes=[])
                            for k in range(4):
                                S.op('dve', lambda v: v.scalar_tensor_tensor(out=eqg[:], in0=Vv[:],
                                                                             scalar=d8[:, k:k + 1], in1=G[:],
                                                                             op0=ALU.is_equal, op1=ALU.mult),
                                     reads=['Vv', 'd8', 'G'], writes=['eqg'])
                                S.op('dve', lambda v: v.tensor_reduce(out=gate_all[:, t128, k:k + 1], in_=eqg[:],
                                                                      axis=AX.X, op=ALU.add),
                                     reads=['eqg'], writes=[f"gate{t128}"])
                        return f

                    def b_ple(s):
                        def f():
                            t128 = it * 2 + s
                            S.pe([lambda t, j=j: t.transpose(out=psT[s][:, j * 128:(j + 1) * 128],
                                                             in_=pb[:, s, j * 128:(j + 1) * 128], identity=ident)
                                  for j in range(2)], reads=['pb'], writes=[f"psT{s}"])
                            S.op('act', lambda a: a.copy(out=pT[:, :, s * 128:(s + 1) * 128],
                                                         in_=psT[s][:, 0:256].rearrange("p (k t) -> p k t", t=128)),
                                 reads=[f"psT{s}"], writes=['pT'])
                            for hf in range(2):
                                S.pe([lambda t, k=k: t.matmul(psB[hf][:], hT[:, k, s * 128:(s + 1) * 128],
                                                              pgw[:, k, hf * 512:(hf + 1) * 512],
                                                              start=(k == 0), stop=(k == 7)) for k in range(8)],
                                     reads=['hT', 'pgw'], writes=[f"psB{hf}"])
                                S.pe([lambda t, j=j: t.matmul(psA[2 + hf][:], pT[:, j, s * 128:(s + 1) * 128],
                                                              ppw[:, j, hf * 512:(hf + 1) * 512],
                                                              start=(j == 0), stop=(j == 1)) for j in range(2)],
                                     reads=['pT', 'ppw'], writes=[f"psA{2 + hf}"])
                                S.op('act', lambda a: a.activation(out=sgm[:, hf * 512:(hf + 1) * 512], in_=psB[hf][:],
                                                                   func=AF.Sigmoid), reads=[f"psB{hf}"], writes=['s_s'])
                                S.op('dve', lambda v: v.tensor_tensor(out=sgm[:, hf * 512:(hf + 1) * 512],
                                                                      in0=sgm[:, hf * 512:(hf + 1) * 512],
                                                                      in1=psA[2 + hf][:], op=ALU.mult),
                                     reads=['s_s', f"psA{2 + hf}"], writes=['s_s'])
                            S.op('dve', lambda v: v.scalar_tensor_tensor(out=rt[0][:], in0=h32[:], scalar=ALPHA,
                                                                         in1=sgm[:], op0=ALU.mult, op1=ALU.add),
                                 reads=['h32', 's_s'], writes=["rt0"])
                            S.dma('sp', "rt0", lambda q: q.dma_start(out=r_buf[t128 * 128:(t128 + 1) * 128, :],
                                                                     in_=rt[0][:]),
                                  reads=["rt0", f"xres{s}"], writes=[])
                        return f
                    return {'BL': b_load, 'PF': b_prefetch, 'M0': b_mix(0), 'M0b': b_mixb(0), 'M1b': b_mixb(1), 'R0a': b_route(0),
                            'R0b': b_route_b(0), 'R0c': b_route_c(0), 'P0': b_ple(0), 'M1': b_mix(1),
                            'R1a': b_route(1), 'R1b': b_route_b(1), 'R1c': b_route_c(1), 'P1': b_ple(1)}

                ORDER = ['BL', 'X', 'M0', 'A0', 'M0b', 'A1', 'A2', 'B0', 'B1', 'R0a', 'C0', 'R0b', 'P0', 'R0c', 'M1',
                         'B2', 'M1b', 'C1', 'C2', 'PL', 'XC2', 'R1a', 'LN', 'R1b', 'P1', 'R1c']
                load_x(0)
                fr = front_steps(0)
                fr['XC']()
                back_steps(0)['PF']()
                for nm in ORDER:
                    if nm in fr:
                        fr[nm]()
                if NT > 1:
                    front_steps(1)['XC']()
                for it in range(NT):
                    fr = front_steps(it + 1) if it + 1 < NT else {}
                    bk = back_steps(it)
                    for nm in ORDER:
                        if nm == 'XC2':
                            if it + 2 < NT:
                                front_steps(it + 2)['XC']()
                        elif nm in bk:
                            bk[nm]()
                        elif nm in fr:
                            fr[nm]()
                    if it + 1 < NT:
                        back_steps(it + 1)['PF']()
                S.barrier()

            with contextlib.ExitStack() as pbk:
                w1 = [sb(f"w1_{i}", [128, 8, 2 * D], BF16, pbk) for i in range(2)]
                w2 = [sb(f"w2_{i}", [128, 8, D], BF16, pbk) for i in range(2)]
                b2b = [sb(f"b2b{i}", [128, D], F32, pbk) for i in range(2)]
                rows = [sb(f"rows{i}", [128, D], BF16, pbk) for i in range(ub)]
                hTg = [sb(f"hTg{i}", [128, 8, US], BF16, pbk) for i in range(2)]
                actT = sb("actT", [128, 8, US], BF16, pbk)
                g2 = [sb(f"g2_{i}", [128, NSPL, NW], F32, pbk) for i in range(2)]
                sgx = [sb(f"sgx{i}", [128, NSPL, NW], F32, pbk) for i in range(2)]
                tg = [sb(f"tg{i}", [128, NSPL, NW], F32, pbk) for i in range(2)]
                au = [sb(f"au{i}", [128, NSPL, NW], F32, pbk) for i in range(2)]
                yst = [sb(f"yst{i}", [128, D], BF16, pbk) for i in range(2)]
                psT = [ps(f"psTb{i}", [128, D], BF16, pbk) for i in range(2)]
                psG = ps("psG", [128, NSPL, 512], F32, pbk)
                psU = ps("psU", [128, NSPL, 512], F32, pbk)
                psY = [ps(f"psY{i}", [128, 512], F32, pbk) for i in range(2)]
                def load_w(e_):
                    ws_ = e_ % 2
                    S.dma('pool', f"w1_{ws_}", lambda q: q.dma_start(
                        out=w1[ws_][:], in_=w1_d[l, e_].rearrange("(k p) n -> p k n", p=128)), writes=[f"w1_{ws_}"])
                    S.dma('pool', f"w2_{ws_}", lambda q: q.dma_start(
                        out=w2[ws_][:], in_=w2_d[l, e_].rearrange("(k p) n -> p k n", p=128)), writes=[f"w2_{ws_}"])
                    S.dma('sp', f"b2b{ws_}", lambda q: q.dma_start(
                        out=b2b[ws_][:], in_=b2_d[l, e_].partition_broadcast(128)), writes=[f"b2b{ws_}"])

                units = [(e, u) for e in range(E) for u in range(NU)]

                def load_rows(ui):
                    e_, u_ = units[ui]
                    base_ = e_ * CAPT + u_ * US
                    for b in range(ub):
                        S.dma('sp', f"rows{b}", lambda q: q.dma_start(
                            out=rows[b][:], in_=hbuf[base_ + b * 128:base_ + (b + 1) * 128, :]),
                            reads=[], writes=[f"rows{b}"])

                def transposes(ui):
                    hsl_ = ui % 2
                    for b in range(ub):
                        pb_ = (ui * ub + b) % 2
                        S.pe([lambda t, k=k: t.transpose(out=psT[pb_][:, k * 128:(k + 1) * 128],
                                                         in_=rows[b][:, k * 128:(k + 1) * 128], identity=ident)
                              for k in range(8)], reads=[f"rows{b}"], writes=[f"psTb{pb_}"])
                        if b % 2:
                            S.op('act', lambda a: a.copy(out=hTg[hsl_][:, :, b * 128:(b + 1) * 128],
                                                         in_=psT[pb_][:].rearrange("p (k t) -> p k t", t=128)),
                                 reads=[f"psTb{pb_}"], writes=[f"hTg{hsl_}_{b}"])
                        else:
                            S.op('dve', lambda v: v.tensor_copy(out=hTg[hsl_][:, :, b * 128:(b + 1) * 128],
                                                                in_=psT[pb_][:].rearrange("p (k t) -> p k t", t=128)),
                                 reads=[f"psTb{pb_}"], writes=[f"hTg{hsl_}_{b}"])

                load_w(0)
                load_rows(0)
                transposes(0)
                for ui, (e, u) in enumerate(units):
                    ws = e % 2
                    hsl = ui % 2
                    base = e * CAPT + u * US
                    hkeys = [f"hTg{hsl}_{b}" for b in range(ub)]
                    if u == 0 and e + 1 < E:
                        load_w(e + 1)
                    if ui + 1 < len(units):
                        load_rows(ui + 1)

                    def fin(fc_):
                        t2 = fc_ % 2
                        S.op('dve', lambda v: v.scalar_tensor_tensor(
                            out=actT[:, fc_, :].rearrange("p (a b) -> p a b", b=NW), in0=au[t2][:], scalar=-6.0,
                            in1=tg[t2][:], op0=ALU.max, op1=ALU.mult),
                            reads=[f"au{t2}", f"tg{t2}"], writes=[f"actT{fc_}"])

                    for fc in range(8):
                        ts2 = fc % 2
                        for (pst, col0, nm) in ((psG, fc * 128, 'psG'), (psU, D + fc * 128, 'psU')):
                            for ns in range(NSPL):
                                S.pe([lambda t, k=k: t.matmul(pst[:, ns, 0:NW], w1[ws][:, k, col0:col0 + 128],
                                                              hTg[hsl][:, k, ns * NW:(ns + 1) * NW],
                                                              start=(k == 0), stop=(k == 7)) for k in range(8)],
                                     reads=[f"w1_{ws}"] + hkeys, writes=[f"{nm}{ns}"])
                        for ns in range(NSPL):
                            S.op('dve', lambda v: v.tensor_scalar(
                                out=g2[ts2][:, ns, :], in0=psG[:, ns, 0:NW],
                                scalar1=b1[:, e * 16 + fc:e * 16 + fc + 1],
                                scalar2=7.0, op0=ALU.add, op1=ALU.min),
                                reads=[f"psG{ns}", 'ppar'], writes=[f"g2_{ts2}_{ns}"])
                        for ns in range(NSPL):
                            S.op('dve', lambda v: v.tensor_scalar(
                                out=au[ts2][:, ns, :], in0=psU[:, ns, 0:NW], scalar1=b1p1[:, e, fc:fc + 1],
                                scalar2=8.0, op0=ALU.add, op1=ALU.min),
                                reads=[f"psU{ns}", 'b1p1'], writes=[f"au{ts2}"])
                        S.op('act', lambda a: a.activation(out=sgx[ts2][:], in_=g2[ts2][:], func=AF.Sigmoid,
                                                           scale=1.702),
                             reads=[f"g2_{ts2}_{ns}" for ns in range(NSPL)], writes=[f"sgx{ts2}"])
                        S.op('pool', lambda g_: g_.tensor_tensor(out=tg[ts2][:], in0=g2[ts2][:], in1=sgx[ts2][:],
                                                                 op=ALU.mult),
                             reads=[f"g2_{ts2}_{ns}" for ns in range(NSPL)] + [f"sgx{ts2}"], writes=[f"tg{ts2}"])
                        if fc > 0:
                            fin(fc - 1)
                    fin(7)
                    if ui + 1 < len(units):
                        transposes(ui + 1)
                    akeys = [f"actT{fc}" for fc in range(8)]
                    for b in range(ub):
                        ysl = b % 2
                        for hf in range(2):
                            S.pe([lambda t, fc=fc: t.matmul(psY[hf][:], actT[:, fc, b * 128:(b + 1) * 128],
                                                            w2[ws][:, fc, hf * 512:(hf + 1) * 512],
                                                            start=(fc == 0), stop=(fc == 7)) for fc in range(8)],
                                 reads=akeys + [f"w2_{ws}"], writes=[f"psY{hf}"])
                            S.op('dve', lambda v: v.tensor_tensor(out=yst[ysl][:, hf * 512:(hf + 1) * 512],
                                                                  in0=psY[hf][:],
                                                                  in1=b2b[ws][:, hf * 512:(hf + 1) * 512], op=ALU.add),
                                 reads=[f"psY{hf}", f"b2b{ws}"], writes=[f"yst{ysl}_{hf}"])
                        S.dma('sp', f"yst{ysl}", lambda q: q.dma_start(
                            out=ybuf[base + b * 128:base + (b + 1) * 128, :], in_=yst[ysl][:]),
                            reads=[f"yst{ysl}_0", f"yst{ysl}_1"], writes=[])
                S.barrier()

            with contextlib.ExitStack() as pd:
                lnbc = sb("lnbc2", [128, 2, D], F32, pd)
                S.dma('sp', 'par_ln', lambda q: q.dma_start(
                    out=lnbc[:].rearrange("p a d -> p (a d)"),
                    in_=lnv_d[l, 2:4].rearrange("a d -> (a d)").partition_broadcast(128)), writes=['lnbc'])
                NS_D = 3
                yk = [sb(f"yk{i}", [128, 4, D], BF16, pd) for i in range(NS_D)]
                acc = [sb(f"acc{i}", [128, D], F32, pd) for i in range(NS_D)]
                ot = [sb(f"ot{i}", [128, D], F32, pd) for i in range(NS_D)]
                st6d = sb("st6d", [128, 2, 6], F32, pd)
                mvd = sb("mvd", [128, 2], F32, pd)
                rsd = sb("rsd", [128, 1], F32, pd)
                nbd = sb("nbd", [128, 1], F32, pd)
                dgG = sb("dgG", [128, 4, 128], BF16, pd)
                psD = [ps(f"psD{i}", [128, 512], F32, pd) for i in range(2)]

                def load_d(t_):
                    sl_ = t_ % NS_D
                    for k in range(4):
                        S.dma('pool', f"yk{sl_}", lambda q: q.indirect_dma_start(
                            out=yk[sl_][:, k, :], out_offset=None, in_=ybuf,
                            in_offset=bass.IndirectOffsetOnAxis(ap=dest_all[:, t_, k:k + 1], axis=0)),
                            reads=[], writes=[f"yk{sl_}_{k}"])
                    S.dma('sp', f"acc{sl_}", lambda q: q.dma_start(out=acc[sl_][:],
                                                                   in_=r_buf[t_ * 128:(t_ + 1) * 128, :]),
                          reads=[], writes=[f"acc{sl_}"])

                for t_ in range(min(NS_D - 1, NT128)):
                    load_d(t_)
                for t in range(NT128):
                    sl = t % NS_D
                    if t + NS_D - 1 < NT128:
                        load_d(t + NS_D - 1)
                    for k in range(4):
                        S.op('dve', lambda v: v.tensor_scalar(out=dgG[:, k, :], in0=ident_f,
                                                              scalar1=gate_all[:, t, k:k + 1], scalar2=None,
                                                              op0=ALU.mult), reads=[], writes=[f"dgG{k}"])
                    for hf in range(2):
                        S.pe([lambda t_, k=k: t_.matmul(psD[hf][:], dgG[:, k, :], yk[sl][:, k, hf * 512:(hf + 1) * 512],
                                                        start=(k == 0), stop=(k == 3)) for k in range(4)],
                             reads=[f"yk{sl}_{kk}" for kk in range(4)] + [f"dgG{kk}" for kk in range(4)],
                             writes=[f"psD{hf}"])
                        S.op('dve', lambda v: v.tensor_tensor(out=acc[sl][:, hf * 512:(hf + 1) * 512],
                                                              in0=psD[hf][:], in1=acc[sl][:, hf * 512:(hf + 1) * 512],
                                                              op=ALU.add),
                             reads=[f"psD{hf}", f"acc{sl}"], writes=[f"acc{sl}"])
                    for hf in range(2):
                        S.op('dve', lambda v: v.bn_stats(out=st6d[:, hf, :], in_=acc[sl][:, hf * 512:(hf + 1) * 512]),
                             reads=[f"acc{sl}"], writes=['st6d'])
                    S.op('dve', lambda v: v.bn_aggr(out=mvd[:], in_=st6d[:].rearrange("p a b -> p (a b)")),
                         reads=['st6d'], writes=['mvd'])
                    S.op('act', lambda a: a.activation(out=rsd[:], in_=mvd[:, 1:2], func=AF.Sqrt, bias=epsc[:, 0:1]),
                         reads=['mvd'], writes=['rsd'])
                    S.op('dve', lambda v: v.reciprocal(out=rsd[:], in_=rsd[:]), reads=['rsd'], writes=['rsd'])
                    S.op('dve', lambda v: v.scalar_tensor_tensor(out=nbd[:], in0=mvd[:, 0:1], scalar=-1.0, in1=rsd[:],
                                                                 op0=ALU.mult, op1=ALU.mult),
                         reads=['mvd', 'rsd'], writes=['nbd'])
                    S.op('act', lambda a: a.activation(out=ot[sl][:], in_=acc[sl][:], func=AF.Identity,
                                                       scale=rsd[:, 0:1], bias=nbd[:, 0:1]),
                         reads=[f"acc{sl}", 'rsd', 'nbd'], writes=[f"ot{sl}"])
                    S.op('dve', lambda v: v.tensor_tensor(out=ot[sl][:], in0=ot[sl][:], in1=lnbc[:, 0, :],
                                                          op=ALU.mult), reads=[f"ot{sl}", 'lnbc'], writes=[f"ot{sl}"])
                    S.op('dve', lambda v: v.tensor_tensor(out=ot[sl][:], in0=ot[sl][:], in1=lnbc[:, 1, :],
                                                          op=ALU.add), reads=[f"ot{sl}", 'lnbc'], writes=[f"ot{sl}"])
                    S.dma('sp', f"ot{sl}", lambda q: q.dma_start(out=dst_x[t * 128:(t + 1) * 128, :], in_=ot[sl][:]),
                          reads=[f"ot{sl}"], writes=[])
                S.barrier()
    return nc


def _consts(capb):
    CAPT = capb * 128
    TRASH1 = E * CAPT + 1
    cf = np.zeros((128, 192), np.float32)
    cf[:, 0:128] = np.eye(128, dtype=np.float32)
    cf[:, 128:160] = (np.arange(E, dtype=np.float32) * CAPT + 1 - TRASH1)[None, :]
    wins = (2, 4, 8, 16)
    for j in range(2):
        for p in range(128):
            w = wins[j * 2 + p // 64]
            for t in range(16):
                cf[p, 160 + j * 16 + t] = w / min(t + 1, w)
    cb = np.zeros((128, 3 * 128 + 4096), np.float32)
    cb[:, 0:128] = np.eye(128)
    cb[:, 128:256] = np.triu(np.ones((128, 128)), 1)
    cb[:, 256:384] = 1.0
    pd = np.zeros((128, 2, 16, 128), np.float32)
    for j in range(2):
        for p in range(128):
            w = wins[j * 2 + p // 64]
            for k in range(w):
                pd[p, j, k, p] = 1.0 / w
    cb[:, 384:] = pd.reshape(128, 4096)
    return cf, cb.astype(ml_dtypes.bfloat16)


def _pack_pp(conv_a_w, conv_a_b, ln_a_g, ln_a_b, conv_b_w, pool_scale, b_gate_up):
    L = conv_a_w.shape[0]
    chan = lambda a, nj: a.reshape(L, nj, 128).transpose(0, 2, 1)
    caw = conv_a_w.transpose(0, 2, 1).reshape(L, 3, 128, 31).transpose(0, 2, 1, 3).reshape(L, 128, 93)
    cbw = conv_b_w.transpose(0, 2, 1).reshape(L, 3, 128, 3).transpose(0, 2, 1, 3).reshape(L, 128, 9)
    b1 = b_gate_up.reshape(L, E, 16, 128).transpose(0, 3, 1, 2).reshape(L, 128, 512)
    return np.ascontiguousarray(np.concatenate(
        [caw, chan(conv_a_b, 3), chan(ln_a_g, 3), chan(ln_a_b, 3), cbw, chan(pool_scale, 2), b1], axis=2),
        dtype=np.float32)


def run(inputs, nseq, depth, capb, ub, ncores=NCORES):
    f = lambda k: np.asarray(inputs[k], dtype=np.float32)
    x, p = f("x"), f("p")
    nc = build_program(nseq, depth, capb, ub)
    cf, cb = _consts(capb)
    pp = _pack_pp(f("conv_a_w")[:depth], f("conv_a_b")[:depth], f("ln_a_g")[:depth], f("ln_a_b")[:depth],
                  f("conv_b_w")[:depth], f("pool_scale")[:depth], f("b_gate_up")[:depth])
    lnv = np.ascontiguousarray(np.stack([f("ln1_g")[:depth], f("ln1_b")[:depth], f("ln2_g")[:depth],
                                         f("ln2_b")[:depth]], axis=1))
    shared = {
        "w_in": f("w_in")[:depth], "w_out": f("w_out")[:depth], "ple_w_gate": f("ple_w_gate")[:depth],
        "ple_w_proj": f("ple_w_proj")[:depth], "router_w": f("router_w")[:depth], "router_b": f("router_b")[:depth],
        "w_gate_up": f("w_gate_up")[:depth], "w_down": f("w_down")[:depth], "b_down": f("b_down")[:depth],
        "pool_w": f("pool_w")[:depth], "lnv": lnv, "pp": pp, "cst_f": cf, "cst_b": cb,
    }
    in_maps = []
    for c in range(ncores):
        m = dict(shared)
        m["x"] = x[c * nseq:(c + 1) * nseq].reshape(nseq * SEQ, D)
        m["p"] = p[:depth, c * nseq:(c + 1) * nseq].reshape(depth, nseq * SEQ, PLE)
        in_maps.append(m)
    res = run_bass_kernel_spmd(nc, in_maps, core_ids=list(range(ncores)))
    out = np.stack([r["out"].reshape(nseq, SEQ, D) for r in res.results], axis=0)
    return out.reshape(ncores * nseq, SEQ, D).astype(np.float32)


def kernel(**inputs):
    return run(inputs, nseq=4, depth=4, capb=10, ub=5)
```
